# Optimizing a Trainium2 kernel written in Bass

```python
import jax, jax.numpy as jnp
from jax import lax
import numpy as np

D_MODEL = 1024
BATCH = 16
SEQ = 2048
DEPTH = 2

CHUNK = 64
N_A_LAYERS = DEPTH // 2
N_B_LAYERS = DEPTH - N_A_LAYERS
N_HEADS = 8
HEAD_DIM = D_MODEL // N_HEADS
ATTN_W = N_HEADS * HEAD_DIM
IDX_HEADS = 8
IDX_DIM = 64
TOPK_MAX = 256
A_QBLOCK = CHUNK
B_QBLOCK = 128
ROPE_THETA = 10000.0
N_EXPERTS = 16
N_GROUPS = 4
EXPERTS_PER_GROUP = N_EXPERTS // N_GROUPS
MOE_TOP_K = 2
D_FF_EXPERT = D_MODEL // 4
DEEPNORM_ALPHA = (2 * DEPTH) ** 0.25
DEEPNORM_BETA = (8 * DEPTH) ** -0.25
LN_EPS = 1e-5
A_SPLITS = (ATTN_W, 2 * ATTN_W, 3 * ATTN_W, 3 * ATTN_W + IDX_HEADS * IDX_DIM,
            3 * ATTN_W + IDX_HEADS * IDX_DIM + IDX_DIM)
A_IN_WIDTH = A_SPLITS[-1] + IDX_HEADS

kernel_name = 'yoco_dsa_stickbreaking_groupmoe_deepnorm'


def layer_norm(x, g, b):
    xf = x.astype(jnp.float32)
    mu = xf.mean(-1, keepdims=True)
    var = jnp.square(xf - mu).mean(-1, keepdims=True)
    y = (xf - mu) * lax.rsqrt(var + LN_EPS) * g.astype(jnp.float32) + b.astype(jnp.float32)
    return y.astype(x.dtype)


def rope_tables(seq, dim):
    inv = 1.0 / (ROPE_THETA ** (jnp.arange(0, dim, 2, dtype=jnp.float32) / dim))
    ang = jnp.arange(seq, dtype=jnp.float32)[:, None] * inv[None, :]
    return jnp.cos(ang), jnp.sin(ang)


def apply_rope(x, cos, sin):
    x1, x2 = jnp.split(x.astype(jnp.float32), 2, axis=-1)
    c = cos[None, :, None, :]
    s = sin[None, :, None, :]
    return jnp.concatenate([x1 * c - x2 * s, x1 * s + x2 * c], axis=-1).astype(x.dtype)


def dsa_mixer(h, w_in, w_out):
    B, S, _ = h.shape
    proj = h @ w_in
    q, k, v, qi, ki, wi = jnp.split(proj, A_SPLITS, axis=-1)
    q = q.reshape(B, S, N_HEADS, HEAD_DIM)
    k = k.reshape(B, S, N_HEADS, HEAD_DIM)
    v = v.reshape(B, S, N_HEADS, HEAD_DIM)
    qi = qi.reshape(B, S, IDX_HEADS, IDX_DIM)
    ki = ki.reshape(B, S, 1, IDX_DIM)
    cos, sin = rope_tables(S, HEAD_DIM)
    q = apply_rope(q, cos, sin)
    k = apply_rope(k, cos, sin)
    ci, si = rope_tables(S, IDX_DIM)
    qi = apply_rope(qi, ci, si)
    ki = apply_rope(ki, ci, si)[:, :, 0]
    wi = wi.astype(jnp.float32) * IDX_HEADS ** -0.5
    topk = min(TOPK_MAX, S // 4)
    s_chunk = jnp.arange(S) // CHUNK

    def block(i):
        t0 = i * A_QBLOCK
        q_b = lax.dynamic_slice_in_dim(q, t0, A_QBLOCK, axis=1)
        qi_b = lax.dynamic_slice_in_dim(qi, t0, A_QBLOCK, axis=1)
        wi_b = lax.dynamic_slice_in_dim(wi, t0, A_QBLOCK, axis=1)
        t_chunk = (t0 + jnp.arange(A_QBLOCK)) // CHUNK
        adm = s_chunk[None, :] <= t_chunk[:, None]
        rel = jax.nn.relu(jnp.einsum('bthd,bsd->bths', qi_b, ki).astype(jnp.float32) * IDX_DIM ** -0.5)
        score = jnp.einsum('bth,bths->bts', wi_b, rel)
        score = jnp.where(adm[None], score, -jnp.inf)
        _, idx = lax.top_k(score, topk)
        valid = (idx // CHUNK) <= t_chunk[None, :, None]
        k_sel = jax.vmap(lambda kb, ib: kb[ib])(k, idx)
        v_sel = jax.vmap(lambda vb, ib: vb[ib])(v, idx)
        logits = jnp.einsum('bthd,btkhd->bhtk', q_b, k_sel).astype(jnp.float32) * HEAD_DIM ** -0.5
        logits = jnp.where(valid[:, None], logits, -jnp.inf)
        p = jax.nn.softmax(logits, axis=-1).astype(v.dtype)
        return jnp.einsum('bhtk,btkhd->bthd', p, v_sel)

    o = lax.map(block, jnp.arange(S // A_QBLOCK))
    o = jnp.moveaxis(o, 0, 1).reshape(B, S, ATTN_W)
    return o @ w_out


def stick_breaking_mixer(h, w_q, k_sb, v_sb, w_out):
    B, S, _ = h.shape
    q = (h @ w_q).reshape(B, S, N_HEADS, HEAD_DIM)
    outs = []
    for i in range(S // B_QBLOCK):
        t0 = i * B_QBLOCK
        t1 = t0 + B_QBLOCK
        z = jnp.einsum('bthd,bshd->bhts', q[:, t0:t1], k_sb[:, :t1]).astype(jnp.float32) * HEAD_DIM ** -0.5
        causal = jnp.arange(t1)[None, :] < jnp.arange(t0, t1)[:, None]
        log_1mb = jnp.where(causal, jax.nn.log_sigmoid(-z), 0.0)
        suffix = lax.cumsum(log_1mb, axis=3, reverse=True) - log_1mb
        a = jnp.where(causal, jnp.exp(jax.nn.log_sigmoid(z) + suffix), 0.0)
        outs.append(jnp.einsum('bhts,bshd->bthd', a.astype(v_sb.dtype), v_sb[:, :t1]))
    o = jnp.concatenate(outs, axis=1).reshape(B, S, ATTN_W)
    return o @ w_out


def grouped_moe(h, router_w, router_bias, w_gate, w_up, w_down):
    B, S, D = h.shape
    xf = h.reshape(-1, D)
    probs = jax.nn.softmax((xf @ router_w).astype(jnp.float32), axis=-1)
    sel = probs + router_bias.astype(jnp.float32)
    grouped = sel.reshape(-1, N_GROUPS, EXPERTS_PER_GROUP)
    gscore = lax.top_k(grouped, 2)[0].sum(-1)
    g = jnp.argmax(gscore, axis=-1)
    in_group = jnp.take_along_axis(grouped, g[:, None, None], axis=1)[:, 0]
    _, local = lax.top_k(in_group, MOE_TOP_K)
    eidx = g[:, None] * EXPERTS_PER_GROUP + local
    w = jnp.take_along_axis(probs, eidx, axis=1)
    w = w / w.sum(-1, keepdims=True)
    gates = (jax.nn.one_hot(eidx, N_EXPERTS, dtype=jnp.float32) * w[..., None]).sum(1)
    gates = gates.astype(xf.dtype)
    y = jnp.zeros_like(xf)
    for e in range(N_EXPERTS):
        he = jax.nn.silu(xf @ w_gate[e]) * (xf @ w_up[e])
        y = y + gates[:, e:e + 1] * (he @ w_down[e])
    return y.reshape(B, S, D)


def setup_inputs(seed: int = 0) -> dict:
    key = jax.random.key(seed)
    ks = jax.random.split(key, 14)
    f32 = jnp.float32
    x = jax.random.normal(ks[0], (BATCH, SEQ, D_MODEL), f32)
    a_col = jnp.ones((A_IN_WIDTH,), f32).at[2 * ATTN_W:3 * ATTN_W].set(DEEPNORM_BETA)
    a_w_in = jax.random.normal(ks[1], (N_A_LAYERS, D_MODEL, A_IN_WIDTH), f32) * D_MODEL ** -0.5 * a_col
    a_w_out = jax.random.normal(ks[2], (N_A_LAYERS, ATTN_W, D_MODEL), f32) * ATTN_W ** -0.5 * DEEPNORM_BETA
    b_w_q = jax.random.normal(ks[3], (N_B_LAYERS, D_MODEL, ATTN_W), f32) * D_MODEL ** -0.5
    kv_col = jnp.ones((2 * ATTN_W,), f32).at[ATTN_W:].set(DEEPNORM_BETA)
    b_w_kv = jax.random.normal(ks[4], (D_MODEL, 2 * ATTN_W), f32) * D_MODEL ** -0.5 * kv_col
    b_w_out = jax.random.normal(ks[5], (N_B_LAYERS, ATTN_W, D_MODEL), f32) * ATTN_W ** -0.5 * DEEPNORM_BETA
    router_w = jax.random.normal(ks[6], (D_MODEL, N_EXPERTS), f32) * D_MODEL ** -0.5
    router_bias = 0.01 * jax.random.normal(ks[7], (N_EXPERTS,), f32)
    exp_w_gate = jax.random.normal(ks[8], (DEPTH, N_EXPERTS, D_MODEL, D_FF_EXPERT), f32) * D_MODEL ** -0.5
    exp_w_up = jax.random.normal(ks[9], (DEPTH, N_EXPERTS, D_MODEL, D_FF_EXPERT), f32) * D_MODEL ** -0.5 * DEEPNORM_BETA
    exp_w_down = jax.random.normal(ks[10], (DEPTH, N_EXPERTS, D_FF_EXPERT, D_MODEL), f32) * D_FF_EXPERT ** -0.5 * DEEPNORM_BETA
    ln_g = 1.0 + 0.01 * jax.random.normal(ks[11], (DEPTH, 2, D_MODEL), f32)
    ln_b = 0.01 * jax.random.normal(ks[12], (DEPTH, 2, D_MODEL), f32)
    return {'x': x, 'a_w_in': a_w_in, 'a_w_out': a_w_out, 'b_w_q': b_w_q, 'b_w_kv': b_w_kv,
            'b_w_out': b_w_out, 'router_w': router_w, 'router_bias': router_bias,
            'exp_w_gate': exp_w_gate, 'exp_w_up': exp_w_up, 'exp_w_down': exp_w_down,
            'ln_g': ln_g, 'ln_b': ln_b}


def reference(x, a_w_in, a_w_out, b_w_q, b_w_kv, b_w_out, router_w, router_bias,
              exp_w_gate, exp_w_up, exp_w_down, ln_g, ln_b):
    B, S, _ = x.shape
    h = x
    k_sb = None
    v_sb = None
    for layer in range(DEPTH):
        if layer < N_A_LAYERS:
            mix = dsa_mixer(h, a_w_in[layer], a_w_out[layer])
        else:
            if layer == N_A_LAYERS:
                kv = h @ b_w_kv
                k_sb, v_sb = jnp.split(kv, 2, axis=-1)
                k_sb = k_sb.reshape(B, S, N_HEADS, HEAD_DIM)
                v_sb = v_sb.reshape(B, S, N_HEADS, HEAD_DIM)
            j = layer - N_A_LAYERS
            mix = stick_breaking_mixer(h, b_w_q[j], k_sb, v_sb, b_w_out[j])
        h = layer_norm(DEEPNORM_ALPHA * h + mix, ln_g[layer, 0], ln_b[layer, 0])
        ffn = grouped_moe(h, router_w, router_bias, exp_w_gate[layer], exp_w_up[layer], exp_w_down[layer])
        h = layer_norm(DEEPNORM_ALPHA * h + ffn, ln_g[layer, 1], ln_b[layer, 1])
    return h
```

```python
import math
from contextlib import ExitStack

import numpy as np

import concourse.bass as bass
import concourse.mybir as mybir
from concourse.bass_utils import run_bass_kernel_spmd

F32 = mybir.dt.float32
BF16 = mybir.dt.bfloat16
AF = mybir.ActivationFunctionType
ALU = mybir.AluOpType
AX = mybir.AxisListType

D = 1024
NH = 8
HD = 128
NE = 16
DFF = 256
ALPHA = 4.0 ** 0.25
LN_EPS = 1e-5
NEG = -30000.0
QK_SCALE = HD ** -0.5
IDX_C = (8 ** -0.5) * (64 ** -0.5)
NIT = 16

ENGS = ("pe", "act", "dve", "pool", "sp")
NRING = 12


class _Op:
    __slots__ = ("id", "eng", "fn", "deps", "is_dma", "ring", "ringval", "marked", "val", "pos")

    def __init__(self, id, eng, fn, is_dma):
        self.id = id
        self.eng = eng
        self.fn = fn
        self.deps = set()
        self.is_dma = is_dma
        self.ring = None
        self.ringval = 0
        self.marked = False
        self.val = 0
        self.pos = 0


class Prog:
    def __init__(self):
        self.ops = []
        self.lastw = {}
        self.readers = {}
        self.eng_ops = {e: [] for e in ENGS}
        self.ring_cnt = {}
        self.ring_last = {}
        self.dma_n = {e: 0 for e in ENGS}
        self._bar_pending = {}
        self._bar_set = ()

    def _add(self, eng, fn, r, w, is_dma=False):
        op = _Op(len(self.ops), eng, fn, is_dma)
        deps = set()
        if self._bar_pending.get(eng):
            self._bar_pending[eng] = False
            deps |= self._bar_set
        for k in r:
            if k in self.lastw:
                deps.add(self.lastw[k])
        for k in w:
            if k in self.lastw:
                deps.add(self.lastw[k])
            deps |= self.readers.get(k, set())
        for k in r:
            self.readers.setdefault(k, set()).add(op.id)
        for k in w:
            self.lastw[k] = op.id
            self.readers[k] = set()
        if is_dma:
            slot = (eng, self.dma_n[eng] % NRING)
            self.dma_n[eng] += 1
            op.ring = slot
            self.ring_cnt[slot] = self.ring_cnt.get(slot, 0) + 1
            op.ringval = 16 * self.ring_cnt[slot]
            if slot in self.ring_last:
                deps.add(self.ring_last[slot])
            self.ring_last[slot] = op.id
        deps.discard(op.id)
        op.deps = deps
        op.pos = len(self.eng_ops[eng])
        self.eng_ops[eng].append(op)
        self.ops.append(op)
        return op

    def barrier(self):
        s = set()
        for e in ENGS:
            if self.eng_ops[e]:
                s.add(self.eng_ops[e][-1].id)
        for slot, oid in self.ring_last.items():
            s.add(oid)
        self._bar_set = s
        self._bar_pending = {e: True for e in ENGS}
        self.lastw = {}
        self.readers = {}

    def pe(self, fn, r=(), w=()):
        return self._add("pe", fn, r, w)

    def act(self, fn, r=(), w=()):
        return self._add("act", fn, r, w)

    def dve(self, fn, r=(), w=()):
        return self._add("dve", fn, r, w)

    def pool(self, fn, r=(), w=()):
        return self._add("pool", fn, r, w)

    def dma(self, fn, r=(), w=(), q="sp"):
        return self._add(q, fn, r, w, is_dma=True)

    def emit(self, nc):
        ops = self.ops
        fin = set()
        for e in ENGS:
            if self.eng_ops[e]:
                fin.add(self.eng_ops[e][-1].id)
        for slot, oid in self.ring_last.items():
            fin.add(oid)
        for op in ops:
            nd = set()
            for d in op.deps:
                dop = ops[d]
                if dop.eng == op.eng and not dop.is_dma and not op.is_dma:
                    if op.eng == "pe":
                        continue
                    if op.pos - dop.pos > 1:
                        continue
                nd.add(d)
            op.deps = nd
        for op in ops:
            for d in op.deps:
                ops[d].marked = True
        for d in fin:
            ops[d].marked = True
        for e in ENGS:
            c = 0
            for op in self.eng_ops[e]:
                if op.is_dma:
                    continue
                if op.marked:
                    c += 1
                    op.val = c
        with ExitStack() as st:
            sems = {e: st.enter_context(nc.semaphore("s_" + e)) for e in ENGS}
            rings = {}
            for slot in self.ring_cnt:
                rings[slot] = st.enter_context(nc.semaphore("r_%s_%d" % slot))
            block = st.enter_context(nc.Block())

            def token(op):
                if op.is_dma:
                    return (rings[op.ring], op.ringval, ("r",) + op.ring)
                return (sems[op.eng], op.val, ("e", op.eng))

            def run(e, eng):
                waited = {}
                for op in self.eng_ops[e]:
                    need = {}
                    for d in op.deps:
                        s, v, key = token(ops[d])
                        if waited.get(key, 0) >= v:
                            continue
                        if need.get(key, (None, 0))[1] < v:
                            need[key] = (s, v)
                    for key, (s, v) in need.items():
                        eng.wait_ge(s, v)
                        waited[key] = v
                    ins = op.fn(eng)
                    if op.is_dma:
                        ins.then_inc(rings[op.ring], 16)
                    elif op.marked:
                        ins.then_inc(sems[e], 1)
                if e == "sp":
                    for d in sorted(fin):
                        s, v, key = token(ops[d])
                        if waited.get(key, 0) >= v:
                            continue
                        eng.wait_ge(s, v)
                        waited[key] = v

            block.tensor(lambda eng: run("pe", eng))
            block.scalar(lambda eng: run("act", eng))
            block.vector(lambda eng: run("dve", eng))
            block.gpsimd(lambda eng: run("pool", eng))
            block.sync(lambda eng: run("sp", eng))


def I(name, *a, **kw):
    return lambda e: getattr(e, name)(*a, **kw)


def G(calls):
    calls = list(calls)

    def fn(e):
        ins = None
        for (name, kw) in calls:
            ins = getattr(e, name)(**kw)
        return ins
    return fn


def MM(out, lhsT, rhs, start=True, stop=True, skip=False):
    kw = dict(out=out, lhsT=lhsT, rhs=rhs, start=start, stop=stop)
    if skip:
        kw["skip_group_check"] = True
    return ("matmul", kw)


class _Stop(Exception):
    pass


class Arena:
    def __init__(self, ap, nwords):
        self.ar = ap
        self.n = nwords
        self.top = 0

    def mark(self):
        return self.top

    def release(self, m):
        self.top = m

    def alloc(self, shape, dt):
        nelem = 1
        for s in shape:
            nelem *= s
        nb = 2 if dt == BF16 else 4
        words = (nelem * nb + 3) // 4
        off = self.top
        self.top += words
        assert self.top <= self.n, "arena overflow %d > %d" % (self.top, self.n)
        v = self.ar[:, off:off + words]
        if dt != F32:
            v = v.bitcast(dt)[:, 0:nelem]
        if len(shape) == 2:
            v = v.rearrange("p (a b) -> p a b", a=shape[0])
        elif len(shape) == 3:
            v = v.rearrange("p (a b c) -> p a b c", a=shape[0], b=shape[1])
        return v


def a_ext_cols():
    cols = []
    q0, k0, v0, qi0, ki0, wi0 = 0, 1024, 2048, 3072, 3584, 3648

    def perm(base, width):
        half = width // 2
        return [base + (i + half) % width for i in range(width)]
    for h in range(8):
        cols += list(range(q0 + h * 128, q0 + (h + 1) * 128))
    for h in range(8):
        cols += perm(q0 + h * 128, 128)
    for h in range(8):
        cols += list(range(k0 + h * 128, k0 + (h + 1) * 128))
    for h in range(8):
        cols += perm(k0 + h * 128, 128)
    for h in range(8):
        cols += list(range(qi0 + h * 64, qi0 + (h + 1) * 64))
    for h in range(8):
        cols += perm(qi0 + h * 64, 64)
    cols += list(range(ki0, ki0 + 64)) * 2
    cols += perm(ki0, 64) * 2
    cols += list(range(v0, v0 + 1024))
    cols += list(range(wi0, wi0 + 8))
    return np.array(cols, dtype=np.int64)


BLK_Q, BLK_QP, BLK_K, BLK_KP, BLK_QI, BLK_QIP, BLK_KI, BLK_KIP = 0, 8, 16, 24, 32, 36, 40, 41
COL_V = 42 * 128
COL_WI = COL_V + 1024
NCOL_EXT = COL_WI + 8


def build(S, NSEQ, TOPK, stop_after=None, debug=False):
    NT = NSEQ * S
    NTL = S // 128
    NG = S // 512
    assert S % 512 == 0
    nc = bass.Bass("TRN2", target_bir_lowering=False)

    def din(name, shape):
        return nc.dram_tensor(name, shape, F32, kind="ExternalInput").ap()
    x = din("x", [NT, D])
    w_in = din("w_in_ext", [D, NCOL_EXT])
    a_w_out = din("a_w_out", [D, D])
    b_w_q = din("b_w_q", [D, D])
    b_w_kv = din("b_w_kv", [D, 2 * D])
    b_w_out = din("b_w_out", [D, D])
    router_w = din("router_w", [D, NE])
    router_b = din("router_bias", [1, NE])
    w_gate = din("exp_w_gate", [2, NE, D, DFF])
    w_up = din("exp_w_up", [2, NE, D, DFF])
    w_down = din("exp_w_down", [2, NE, DFF, D])
    ln_g = din("ln_g", [4, D])
    ln_b = din("ln_b", [4, D])
    c_ident = din("c_ident", [128, 128])
    c_tri = din("c_tri", [128, 128])
    c_caus = din("c_caus", [128, 128])
    c_rope = din("c_rope", [4, 128, S])
    out = nc.dram_tensor("out", [NT, D], F32, kind="ExternalOutput").ap()
    skind = "ExternalOutput" if debug else "Internal"
    h1 = nc.dram_tensor("h1", [NT, D], F32, kind=skind).ap()
    h2 = nc.dram_tensor("h2", [NT, D], F32, kind=skind).ap()
    h3 = nc.dram_tensor("h3", [NT, D], F32, kind=skind).ap()
    qT_d = nc.dram_tensor("qT_d", [8, 128, S], BF16, kind="Internal").ap()
    qiT_d = nc.dram_tensor("qiT_d", [4, 128, S], BF16, kind="Internal").ap()

    P = Prog()
    es = ExitStack()
    ARW = 51800
    arena_t = es.enter_context(nc.sbuf_tensor("arena", [128, ARW], F32))
    ar = Arena(arena_t, ARW)
    psum_t = es.enter_context(nc.psum_tensor("psum", [128, 8, 512], F32))
    PS = [psum_t[:, b, :] for b in range(8)]
    PSB = [psum_t[:, b, :].bitcast(BF16) for b in range(8)]

    def psk(b):
        return ("ps", b)

    identF = ar.alloc([128], F32)
    identB = ar.alloc([128], BF16)
    onesB = ar.alloc([128], BF16)
    onesF = ar.alloc([128], F32)
    triB = ar.alloc([128], BF16)
    causF = ar.alloc([128], F32)
    u2B = ar.alloc([128], BF16)
    ctmp = ar.alloc([128], F32)
    rw32 = ar.alloc([8, NE], F32)
    rbias = ar.alloc([NE], F32)
    pw = ar.alloc([NIT], F32)
    sel8 = ar.alloc([4, 8], F32)
    epsT = ar.alloc([1], F32)
    P.dma(I("dma_start", out=identF, in_=c_ident), w=["identF"])
    P.dve(I("tensor_copy", out=identB, in_=identF), r=["identF"], w=["identB"])
    P.dve(I("memset", onesB, 1.0), w=["onesB"])
    P.dve(I("memset", onesF, 1.0), w=["onesF"])
    P.dma(I("dma_start", out=ctmp, in_=c_tri), w=["ctmp"])
    P.dve(I("tensor_copy", out=triB, in_=ctmp), r=["ctmp"], w=["triB"])
    P.dma(I("dma_start", out=causF, in_=c_caus), w=["causF"])
    P.dve(I("tensor_copy", out=u2B, in_=causF), r=["causF"], w=["u2B"])
    P.dma(I("dma_start", out=rw32, in_=router_w.rearrange("(k p) e -> p k e", p=128)), w=["rw32"])
    P.dma(I("dma_start", out=rbias, in_=router_b.broadcast_to([128, NE])), w=["rbias"])
    for it in range(NIT):
        P.dve(I("memset", pw[:, it:it + 1], 2.0 ** -(it + 1)), w=["pw"])
    P.dve(I("memset", sel8, -1e30), w=["sel8"])
    P.dve(I("memset", epsT, LN_EPS), w=["epsT"])
    base_mark = ar.mark()
    P.barrier()

    def layer_norm(pre, kpre, yo, kyo, gb, small, kidx):
        bst, mv, lnv, rstd, nmr = small
        ks = ("lnsm", kidx)
        P.dve(I("bn_stats", out=bst[:, 0, :], in_=pre[:, 0:512]), r=[kpre], w=[ks])
        P.dve(I("bn_stats", out=bst[:, 1, :], in_=pre[:, 512:1024]), r=[kpre], w=[ks])
        P.dve(I("bn_aggr", out=mv, in_=bst), r=[ks], w=[ks])
        P.act(I("activation", out=lnv, in_=mv[:, 1:2], func=AF.Ln, bias=epsT[:, 0:1], scale=1.0), r=[ks, "epsT"], w=[ks])
        P.act(I("activation", out=rstd, in_=lnv, func=AF.Exp, scale=-0.5), r=[ks], w=[ks])
        P.dve(I("tensor_scalar", out=nmr, in0=mv[:, 0:1], scalar1=rstd[:, 0:1], scalar2=-1.0, op0=ALU.mult, op1=ALU.mult), r=[ks], w=[ks])
        P.act(I("activation", out=pre, in_=pre, func=AF.Identity, scale=rstd[:, 0:1], bias=nmr[:, 0:1]), r=[ks], w=[kpre])
        P.pool(I("tensor_tensor", out=pre, in0=pre, in1=gb[:, 0, :], op=ALU.mult), r=["gb"], w=[kpre])
        P.pool(I("tensor_tensor", out=yo, in0=pre, in1=gb[:, 1, :], op=ALU.add), r=["gb", kpre], w=[kyo])

    def ln_smalls():
        return (ar.alloc([2, 6], F32), ar.alloc([2], F32), ar.alloc([1], F32), ar.alloc([1], F32), ar.alloc([1], F32))

    def load_gb(gb, idx):
        P.dma(I("dma_start", out=gb[:, 0, :], in_=ln_g[idx:idx + 1, :].broadcast_to([128, D])), w=["gb"])
        P.dma(I("dma_start", out=gb[:, 1, :], in_=ln_b[idx:idx + 1, :].broadcast_to([128, D])), w=["gb"])

    def build_xT(src, tok0, BA, xs):
        for i in range(NTL):
            xb = xs[i % len(xs)]
            kx = ("xs", i % len(xs))
            P.dma(I("dma_start", out=xb, in_=src[tok0 + i * 128: tok0 + (i + 1) * 128, :]), w=[kx])
            ba, bb = (0, 1) if i % 2 == 0 else (2, 3)
            calls = []
            for k in range(8):
                bank = ba if k < 4 else bb
                calls.append(("transpose", dict(out=PS[bank][:, (k % 4) * 128:(k % 4 + 1) * 128], in_=xb[:, k * 128:(k + 1) * 128], identity=identF)))
            P.pe(G(calls), r=[kx, "identF"], w=[psk(ba), psk(bb)])
            P.act(I("copy", out=BA[:, 0:4, i * 128:(i + 1) * 128], in_=PS[ba].rearrange("p (a b) -> p a b", a=4)), w=[psk(ba), ("BA", i, 0)])
            P.dve(I("tensor_copy", out=BA[:, 4:8, i * 128:(i + 1) * 128], in_=PS[bb].rearrange("p (a b) -> p a b", a=4)), w=[psk(bb), ("BA", i, 1)])

    def ba_keys(t0, t1):
        ks = []
        for i in range(t0 // 128, (t1 + 127) // 128):
            ks += [("BA", i, 0), ("BA", i, 1)]
        return ks

    def load_wcol(dst, key, wap, c0):
        P.dma(I("dma_start", out=dst, in_=wap[:, c0:c0 + 128].rearrange("(k p) c -> p k c", p=128)), w=[key], q="pool")

    def out_proj_ln(BA, WBIG, src, dst, tok0, lnidx, xs, pres, gb, smalls):
        NX, NP_ = len(xs), len(pres)

        def s0(i):
            xb, kx = xs[i % NX], ("xs", i % NX)
            pre, kp = pres[i % NP_], ("pre", i % NP_)
            bst, mv, lnv, rstd, nmr = smalls[i % NP_]
            ks = ("lnsm", i % NP_)
            P.dma(I("dma_start", out=xb, in_=src[tok0 + i * 128: tok0 + (i + 1) * 128, :]), w=[kx])
            for c in range(2):
                bank = (i % 2) * 2 + c
                P.pe(G([MM(PS[bank], BA[:, k, i * 128:(i + 1) * 128], WBIG[:, k, c * 512:(c + 1) * 512], start=(k == 0), stop=(k == 7)) for k in range(8)]),
                     r=["oT", "WBIG"], w=[psk(bank)])
                P.dve(I("scalar_tensor_tensor", out=pre[:, c * 512:(c + 1) * 512], in0=xb[:, c * 512:(c + 1) * 512], scalar=ALPHA, in1=PS[bank], op0=ALU.mult, op1=ALU.add),
                      r=[kx], w=[psk(bank), (kp, c)])
                P.dve(I("bn_stats", out=bst[:, c, :], in_=pre[:, c * 512:(c + 1) * 512]), r=[(kp, c)], w=[(ks, "b", c)])
            P.dve(I("bn_aggr", out=mv, in_=bst), r=[(ks, "b", 0), (ks, "b", 1)], w=[(ks, "mv")])

        def s1(i):
            bst, mv, lnv, rstd, nmr = smalls[i % NP_]
            ks = ("lnsm", i % NP_)
            P.act(I("activation", out=lnv, in_=mv[:, 1:2], func=AF.Ln, bias=epsT[:, 0:1], scale=1.0), r=[(ks, "mv"), "epsT"], w=[(ks, "ln")])
            P.act(I("activation", out=rstd, in_=lnv, func=AF.Exp, scale=-0.5), r=[(ks, "ln")], w=[(ks, "rstd")])

        def s2(i):
            pre, kp = pres[i % NP_], ("pre", i % NP_)
            bst, mv, lnv, rstd, nmr = smalls[i % NP_]
            ks = ("lnsm", i % NP_)
            P.dve(I("tensor_scalar", out=nmr, in0=mv[:, 0:1], scalar1=rstd[:, 0:1], scalar2=-1.0, op0=ALU.mult, op1=ALU.mult), r=[(ks, "mv"), (ks, "rstd")], w=[(ks, "nmr")])
            P.act(I("activation", out=pre, in_=pre, func=AF.Identity, scale=rstd[:, 0:1], bias=nmr[:, 0:1]), r=[(ks, "rstd"), (ks, "nmr")], w=[(kp, 0), (kp, 1)])

        def s3(i):
            pre, kp = pres[i % NP_], ("pre", i % NP_)
            P.pool(I("tensor_tensor", out=pre, in0=pre, in1=gb[:, 0, :], op=ALU.mult), r=["gb"], w=[(kp, 0), (kp, 1)])
            P.pool(I("tensor_tensor", out=pre, in0=pre, in1=gb[:, 1, :], op=ALU.add), r=["gb"], w=[(kp, 0), (kp, 1)])
            P.dma(I("dma_start", out=dst[tok0 + i * 128: tok0 + (i + 1) * 128, :], in_=pre), r=[(kp, 0), (kp, 1)], w=[("dst", i)])

        for n in range(NTL + 3):
            if n < NTL:
                s0(n)
            if 0 <= n - 1 < NTL:
                s1(n - 1)
            if 0 <= n - 2 < NTL:
                s2(n - 2)
            if 0 <= n - 3 < NTL:
                s3(n - 3)

    def mixer_A(b):
        tok0 = b * S
        ar.release(base_mark)
        BK = ar.alloc([8, S], BF16)
        BV = ar.alloc([NTL, D], BF16)
        BKI = ar.alloc([S], BF16)
        BA = ar.alloc([8, S], BF16)
        wiall = ar.alloc([NTL, 8], F32)
        aw = ar.alloc([NTL, 8], F32)
        sg = ar.alloc([NTL, 8], F32)
        m2 = ar.mark()
        WBIG = ar.alloc([8, D], BF16)
        rope = ar.alloc([2, S], F32)
        WC = [ar.alloc([8, 128], BF16) for _ in range(4)]
        Wwi = ar.alloc([8, 8], BF16)
        xs = [ar.alloc([D], F32) for _ in range(2)]
        t1 = [ar.alloc([512], F32) for _ in range(2)]
        t2 = [ar.alloc([512], F32) for _ in range(2)]
        ob = [ar.alloc([512], BF16) for _ in range(2)]

        build_xT(x, tok0, BA, xs)
        P.dma(I("dma_start", out=WBIG, in_=w_in[:, COL_V:COL_V + D].rearrange("(k p) c -> p k c", p=128)), w=["WBIG"], q="pool")
        P.dma(I("dma_start", out=Wwi, in_=w_in[:, COL_WI:COL_WI + 8].rearrange("(k p) c -> p k c", p=128)), w=["Wwi"], q="pool")

        blocks = []
        for j in range(4):
            blocks.append((BLK_QI + j, BLK_QIP + j, 1, ("qi", j)))
        blocks.append((BLK_KI, BLK_KIP, 1, ("ki", 0)))
        for h in range(8):
            blocks.append((BLK_Q + h, BLK_QP + h, 0, ("q", h)))
        for h in range(8):
            blocks.append((BLK_K + h, BLK_KP + h, 0, ("k", h)))
        cur_fam = None
        n = 0
        for bi, (ba_, bp_, fam, dest) in enumerate(blocks):
            if fam != cur_fam:
                cur_fam = fam
                t_c, t_s = (2, 3) if fam == 1 else (0, 1)
                P.dma(I("dma_start", out=rope[:, 0, :], in_=c_rope[t_c]), w=["rope"])
                P.dma(I("dma_start", out=rope[:, 1, :], in_=c_rope[t_s]), w=["rope"])
            wa, wp = WC[2 * (bi % 2)], WC[2 * (bi % 2) + 1]
            ka, kp_ = ("WC", 2 * (bi % 2)), ("WC", 2 * (bi % 2) + 1)
            if bi == 0:
                load_wcol(wa, ka, w_in, ba_ * 128)
                load_wcol(wp, kp_, w_in, bp_ * 128)
            if bi + 1 < len(blocks):
                nb1 = (bi + 1) % 2
                load_wcol(WC[2 * nb1], ("WC", 2 * nb1), w_in, blocks[bi + 1][0] * 128)
                load_wcol(WC[2 * nb1 + 1], ("WC", 2 * nb1 + 1), w_in, blocks[bi + 1][1] * 128)
            for tg in range(NG):
                c0, c1 = tg * 512, (tg + 1) * 512
                pa, pb = (0, 1) if n % 2 == 0 else (2, 3)
                j2 = n % 2
                n += 1
                P.pe(G([MM(PS[pa], wa[:, k, :], BA[:, k, c0:c1], start=(k == 0), stop=(k == 7)) for k in range(8)]), r=[ka] + ba_keys(c0, c1), w=[psk(pa)])
                P.pe(G([MM(PS[pb], wp[:, k, :], BA[:, k, c0:c1], start=(k == 0), stop=(k == 7)) for k in range(8)]), r=[kp_] + ba_keys(c0, c1), w=[psk(pb)])
                P.dve(I("tensor_tensor", out=t1[j2], in0=PS[pa], in1=rope[:, 0, c0:c1], op=ALU.mult), r=["rope"], w=[psk(pa), ("t1", j2)])
                P.dve(I("tensor_tensor", out=t2[j2], in0=PS[pb], in1=rope[:, 1, c0:c1], op=ALU.mult), r=["rope"], w=[psk(pb), ("t2", j2)])
                kind, idx = dest
                if kind == "k":
                    P.pool(I("tensor_tensor", out=BK[:, idx, c0:c1], in0=t1[j2], in1=t2[j2], op=ALU.add), r=[("t1", j2), ("t2", j2)], w=[("BK", idx)])
                elif kind == "ki":
                    P.pool(I("tensor_tensor", out=BKI[:, c0:c1], in0=t1[j2], in1=t2[j2], op=ALU.add), r=[("t1", j2), ("t2", j2)], w=["BKI"])
                else:
                    P.pool(I("tensor_tensor", out=ob[j2], in0=t1[j2], in1=t2[j2], op=ALU.add), r=[("t1", j2), ("t2", j2)], w=[("ob", j2)])
                    dd = qT_d if kind == "q" else qiT_d
                    P.dma(I("dma_start", out=dd[idx, :, c0:c1], in_=ob[j2]), r=[("ob", j2)], w=[(kind + "d", tg)])
        if stop_after == "A.1":
            raise _Stop()
        for i in range(NTL):
            for c in range(2):
                bank = 4 + (2 * i + c) % 4
                P.pe(G([MM(PS[bank], BA[:, k, i * 128:(i + 1) * 128], WBIG[:, k, c * 512:(c + 1) * 512], start=(k == 0), stop=(k == 7)) for k in range(8)]),
                     r=ba_keys(i * 128, (i + 1) * 128) + ["WBIG"], w=[psk(bank)])
                if c == 0:
                    P.act(I("copy", out=BV[:, i, 0:512], in_=PS[bank]), w=[psk(bank), ("BV", i)])
                else:
                    P.dve(I("tensor_copy", out=BV[:, i, 512:1024], in_=PS[bank]), w=[psk(bank), ("BV", i)])
            bank = i % 2
            P.pe(G([MM(PS[bank][:, 0:8], BA[:, k, i * 128:(i + 1) * 128], Wwi[:, k, :], start=(k == 0), stop=(k == 7)) for k in range(8)]),
                 r=ba_keys(i * 128, (i + 1) * 128) + ["Wwi"], w=[psk(bank)])
            P.dve(I("tensor_copy", out=wiall[:, i, :], in_=PS[bank][:, 0:8]), w=[psk(bank), "wiall"])
        P.dve(I("tensor_scalar", out=sg, in0=wiall, scalar1=-IDX_C, scalar2=None, op0=ALU.mult), r=["wiall"], w=["sg"])
        P.dve(I("scalar_tensor_tensor", out=aw, in0=wiall, scalar=IDX_C, in1=sg, op0=ALU.mult, op1=ALU.max), r=["wiall", "sg"], w=["aw"])
        P.dve(I("tensor_scalar", out=sg, in0=wiall, scalar1=0.0, scalar2=2.0, op0=ALU.is_ge, op1=ALU.mult), r=["wiall"], w=["sg"])
        P.dve(I("tensor_scalar", out=sg, in0=sg, scalar1=-1.0, scalar2=None, op0=ALU.add), w=["sg"])

        if stop_after == "A.2":
            raise _Stop()
        P.barrier()
        ar.release(m2)
        qg = [ar.alloc([8, 512], BF16) for _ in range(2)]
        qig = [ar.alloc([4, 512], BF16) for _ in range(2)]
        maskT = [ar.alloc([NTL, 512], BF16) for _ in range(2)]
        score = [ar.alloc([S], F32) for _ in range(2)]
        MB = [ar.alloc([S], BF16) for _ in range(2)]
        Rt = [ar.alloc([512], BF16) for _ in range(4)]
        Dg = [ar.alloc([8, 128], BF16) for _ in range(2)]
        PT = [ar.alloc([512], BF16) for _ in range(3)]
        U = ar.alloc([512], F32)
        Vs = ar.alloc([512], F32)
        bsm = [dict(rmax=ar.alloc([1], F32), rmin=ar.alloc([1], F32), rng=ar.alloc([1], F32), dtab=ar.alloc([NIT], F32),
                    cand=ar.alloc([1], F32), cnt=ar.alloc([1], F32), step=ar.alloc([1], F32), cur=ar.alloc([1], F32)) for _ in range(2)]
        cnts = {"R": 0, "S": 0, "P": 0, "Rt": 0}

        def load_group(g):
            P.dma(I("dma_start", out=qg[g % 2], in_=qT_d[:, :, g * 512:(g + 1) * 512].rearrange("h p s -> p h s")), w=[("qg", g % 2)])
            P.dma(I("dma_start", out=qig[g % 2], in_=qiT_d[:, :, g * 512:(g + 1) * 512].rearrange("h p s -> p h s")), w=[("qig", g % 2)])

        def idx_scores(g, j):
            qigb = qig[g % 2]
            i = 4 * g + j
            L1, L2 = 128 * i + 64, 128 * i + 128
            sc = score[j % 2]
            ksc = ("score", j % 2)
            dg = Dg[j % 2]
            kdg = ("Dg", j % 2)
            P.dve(I("tensor_tensor", out=dg, in0=identB.unsqueeze(1).to_broadcast([128, 8, 128]), in1=sg[:, i, :].unsqueeze(2).to_broadcast([128, 8, 128]), op=ALU.mult),
                  r=["identB", "sg"], w=[kdg])
            steps = []
            for kr in range(0, L2, 512):
                for h in range(8):
                    steps.append((kr, min(512, L2 - kr), h))
            info = {}

            def rel(n):
                kr, w_, h = steps[n]
                hp = (h % 2) * 64
                bank = cnts["R"] % 2
                cnts["R"] += 1
                info[n] = bank
                P.pe(I("matmul", out=PS[bank][:, 0:w_], lhsT=qigb[hp:hp + 64, h // 2, j * 128:(j + 1) * 128], rhs=BKI[hp:hp + 64, kr:kr + w_], start=True, stop=True),
                     r=[("qig", g % 2), "BKI"], w=[psk(bank)])

            def relu_acc(n):
                kr, w_, h = steps[n]
                bank = info[n]
                rt = Rt[cnts["Rt"] % 4]
                krt = ("Rt", cnts["Rt"] % 4)
                cnts["Rt"] += 1
                P.act(I("activation", out=rt[:, 0:w_], in_=PS[bank][:, 0:w_], func=AF.Relu, scale=aw[:, i, h:h + 1]), r=["aw"], w=[psk(bank), krt])
                P.pe(I("matmul", out=PS[7][:, 0:w_], lhsT=dg[:, h, :], rhs=rt[:, 0:w_], start=(h == 0), stop=(h == 7)), r=[krt, kdg], w=[psk(7)])
                if h == 7:
                    P.dve(I("tensor_copy", out=sc[:, kr:kr + w_], in_=PS[7][:, 0:w_]), w=[psk(7), ksc])
            rel(0)
            for n in range(len(steps)):
                if n + 1 < len(steps):
                    rel(n + 1)
                relu_acc(n)
            P.dve(I("memset", sc[0:64, L1:L2], -3.0e38), w=[ksc])

        def bis_ops(g, j):
            i = 4 * g + j
            L1, L2 = 128 * i + 64, 128 * i + 128
            sc, mb, sm = score[j % 2], MB[j % 2], bsm[j % 2]
            ksc, kb_, kmb, kcur = ("score", j % 2), ("bis", j % 2), ("MB", j % 2), ("cur", j % 2)
            ops = []
            if L1 <= TOPK:
                ops.append(lambda: P.dve(I("memset", sm["cur"], -1.0e30), w=[kcur]))
                return ops
            ops.append(lambda: P.dve(I("tensor_reduce", out=sm["rmax"], in_=sc[:, 0:L2], axis=AX.X, op=ALU.max), r=[ksc], w=[kb_]))
            ops.append(lambda: P.dve(I("tensor_reduce", out=sm["rmin"], in_=sc[:, 0:L1], axis=AX.X, op=ALU.min), r=[ksc], w=[kb_]))
            ops.append(lambda: P.dve(I("tensor_tensor", out=sm["rng"], in0=sm["rmax"], in1=sm["rmin"], op=ALU.subtract), w=[kb_]))
            ops.append(lambda: P.dve(I("tensor_scalar", out=sm["dtab"], in0=pw, scalar1=sm["rng"][:, 0:1], scalar2=None, op0=ALU.mult), r=["pw"], w=[kb_]))
            ops.append(lambda: P.dve(I("tensor_tensor", out=sm["cand"], in0=sm["rmin"], in1=sm["dtab"][:, 0:1], op=ALU.add), w=[kb_]))
            for it in range(NIT):
                ops.append(lambda: P.dve(I("tensor_scalar", out=mb[:, 0:L2], in0=sc[:, 0:L2], scalar1=sm["cand"][:, 0:1], scalar2=0.0, op0=ALU.is_ge, op1=ALU.add, accum_out=sm["cnt"]),
                                         r=[ksc], w=[kb_, kmb]))
                ops.append(lambda it=it: P.dve(I("tensor_scalar", out=sm["step"], in0=sm["cnt"], scalar1=float(TOPK) - 0.5, scalar2=sm["dtab"][:, it:it + 1], op0=ALU.is_ge, op1=ALU.mult), w=[kb_]))
                if it < NIT - 1:
                    ops.append(lambda it=it: P.dve(I("scalar_tensor_tensor", out=sm["cand"], in0=sm["step"], scalar=sm["dtab"][:, it + 1:it + 2], in1=sm["cand"], op0=ALU.subtract, op1=ALU.add), w=[kb_]))
                else:
                    ops.append(lambda it=it: P.dve(I("scalar_tensor_tensor", out=sm["cur"], in0=sm["step"], scalar=sm["dtab"][:, it:it + 1], in1=sm["cand"], op0=ALU.subtract, op1=ALU.add), w=[kb_, kcur]))
            return ops

        def mask_tile(g, j):
            i = 4 * g + j
            L2 = 128 * i + 128
            sc, mb, sm = score[j % 2], MB[j % 2], bsm[j % 2]
            mT = maskT[g % 2]
            P.dve(I("tensor_scalar", out=mb[:, 0:L2], in0=sc[:, 0:L2], scalar1=sm["cur"][:, 0:1], scalar2=NEG, op0=ALU.is_lt, op1=ALU.mult),
                  r=[("score", j % 2), ("cur", j % 2)], w=[("MB", j % 2)])
            for kb0 in range(0, i + 1, 4):
                nb = min(4, i + 1 - kb0)
                bank = cnts["R"] % 2
                cnts["R"] += 1
                P.pe(G([("transpose", dict(out=PSB[bank][:, q * 128:(q + 1) * 128], in_=mb[:, (kb0 + q) * 128:(kb0 + q + 1) * 128], identity=identB)) for q in range(nb)]),
                     r=[("MB", j % 2), "identB"], w=[psk(bank)])
                P.act(I("copy", out=mT[:, kb0:kb0 + nb, j * 128:(j + 1) * 128], in_=PSB[bank][:, 0:nb * 128].rearrange("p (a b) -> p a b", a=nb)),
                      w=[psk(bank), ("maskT", g % 2, j)])

        def att_heads(g, heads):
            qgb = qg[g % 2]
            mT = maskT[g % 2]
            nkb = 4 * g + 4
            bo, bs = 5, 6
            blks = [(h, kb) for h in heads for kb in range(nkb)]
            info = {}

            def sS(n):
                h, kb = blks[n]
                jl = max(0, kb - 4 * g)
                q0 = jl * 128
                N = 512 - q0
                sb_ = 2 + (cnts["S"] % 3)
                cnts["S"] += 1
                info[n] = sb_
                P.pe(G([MM(PS[sb_][:, 0:N], BK[:, h, kb * 128:(kb + 1) * 128], qgb[:, h, q0:512], start=True, stop=False),
                        MM(PS[sb_][:, 0:N], identB, mT[:, kb, q0:512], start=False, stop=True)]),
                     r=[("BK", h), ("qg", g % 2), "identB"] + [("maskT", g % 2, jj) for jj in range(jl, 4)], w=[psk(sb_)])

            def sPV(n):
                h, kb = blks[n]
                jl = max(0, kb - 4 * g)
                q0 = jl * 128
                N = 512 - q0
                sb_ = info[n]
                pt = PT[cnts["P"] % 3]
                kpt = ("PT", cnts["P"] % 3)
                cnts["P"] += 1
                P.act(I("activation", out=pt[:, 0:N], in_=PS[sb_][:, 0:N], func=AF.Exp, scale=QK_SCALE), w=[psk(sb_), kpt])
                P.pe(G([MM(PS[bo][:, q0:512], BV[:, kb, h * 128:(h + 1) * 128], pt[:, 0:N], start=(kb == 0), stop=(kb == nkb - 1)),
                        MM(PS[bs][:, q0:512], onesB, pt[:, 0:N], start=(kb == 0), stop=(kb == nkb - 1))]),
                     r=[kpt, ("BV", kb), "onesB"], w=[psk(bo), psk(bs)])
                if kb == nkb - 1:
                    P.act(I("copy", out=U, in_=PS[bo]), w=[psk(bo), "U"])
                    P.act(I("activation", out=Vs, in_=PS[bs], func=AF.Ln), w=[psk(bs), "Vs"])
                    P.act(I("activation", out=Vs, in_=Vs, func=AF.Exp, scale=-1.0), w=["Vs"])
                    P.pool(I("tensor_tensor", out=BA[:, h, g * 512:(g + 1) * 512], in0=U, in1=Vs, op=ALU.mult), r=["U", "Vs"], w=["oT"])
            sS(0)
            for n in range(len(blks)):
                if n + 1 < len(blks):
                    sS(n + 1)
                sPV(n)

        for g in range(NG + 1):
            if g < NG:
                load_group(g)
            for pr in range(2):
                if g < NG:
                    idx_scores(g, 2 * pr)
                    idx_scores(g, 2 * pr + 1)
                    la, lb = bis_ops(g, 2 * pr), bis_ops(g, 2 * pr + 1)
                    for k in range(max(len(la), len(lb))):
                        if k < len(la):
                            la[k]()
                        if k < len(lb):
                            lb[k]()
                if g >= 1:
                    att_heads(g - 1, list(range(4 * pr, 4 * pr + 4)))
                if g < NG:
                    mask_tile(g, 2 * pr)
                    mask_tile(g, 2 * pr + 1)
        if stop_after == "A.3":
            raise _Stop()
        P.barrier()
        ar.release(m2)
        WBIG = ar.alloc([8, D], BF16)
        P.dma(I("dma_start", out=WBIG, in_=a_w_out.rearrange("(k p) c -> p k c", p=128)), w=["WBIG"], q="pool")
        gb = ar.alloc([2, D], F32)
        load_gb(gb, 0)
        xs2 = [ar.alloc([D], F32) for _ in range(3)]
        pres = [ar.alloc([D], F32) for _ in range(5)]
        smalls = [ln_smalls() for _ in range(5)]
        out_proj_ln(BA, WBIG, x, h1, tok0, 0, xs2, pres, gb, smalls)
        P.barrier()

    def mixer_B(b):
        tok0 = b * S
        ar.release(base_mark)
        BK = ar.alloc([8, S], BF16)
        BQ = ar.alloc([8, S], BF16)
        BV = ar.alloc([NTL, D], BF16)
        BA = ar.alloc([8, S], BF16)
        WBIG = ar.alloc([8, D], BF16)
        m2 = ar.mark()
        WC = [ar.alloc([8, 128], BF16) for _ in range(2)]
        xs = [ar.alloc([D], F32) for _ in range(3)]
        build_xT(h2, tok0, BA, xs)
        P.dma(I("dma_start", out=WBIG, in_=b_w_kv[:, D:2 * D].rearrange("(k p) c -> p k c", p=128)), w=["WBIG"], q="pool")
        n = 0
        for bi in range(16):
            wap, c0w, dstB, kd = (b_w_q, bi * 128, BQ, ("BQ", bi)) if bi < 8 else (b_w_kv, (bi - 8) * 128, BK, ("BK", bi - 8))
            wa, ka = WC[bi % 2], ("WC", bi % 2)
            if bi == 0:
                load_wcol(wa, ka, wap, c0w)
            if bi + 1 < 16:
                bn = bi + 1
                wapn, c0n = (b_w_q, bn * 128) if bn < 8 else (b_w_kv, (bn - 8) * 128)
                load_wcol(WC[bn % 2], ("WC", bn % 2), wapn, c0n)
            for tg in range(NG):
                c0, c1 = tg * 512, (tg + 1) * 512
                pa = n % 4
                n += 1
                P.pe(G([MM(PS[pa], wa[:, k, :], BA[:, k, c0:c1], start=(k == 0), stop=(k == 7)) for k in range(8)]), r=[ka] + ba_keys(c0, c1), w=[psk(pa)])
                if n % 2 == 0:
                    P.act(I("copy", out=dstB[:, bi % 8, c0:c1], in_=PS[pa]), w=[psk(pa), kd])
                else:
                    P.dve(I("tensor_copy", out=dstB[:, bi % 8, c0:c1], in_=PS[pa]), w=[psk(pa), kd])
        for i in range(NTL):
            for c in range(2):
                bank = 4 + (2 * i + c) % 4
                P.pe(G([MM(PS[bank], BA[:, k, i * 128:(i + 1) * 128], WBIG[:, k, c * 512:(c + 1) * 512], start=(k == 0), stop=(k == 7)) for k in range(8)]),
                     r=ba_keys(i * 128, (i + 1) * 128) + ["WBIG"], w=[psk(bank)])
                if c == 0:
                    P.act(I("copy", out=BV[:, i, 0:512], in_=PS[bank]), w=[psk(bank), ("BV", i)])
                else:
                    P.dve(I("tensor_copy", out=BV[:, i, 512:1024], in_=PS[bank]), w=[psk(bank), ("BV", i)])
        P.barrier()
        ar.release(m2)
        NEB = 6
        E = [ar.alloc([512], F32) for _ in range(NEB)]
        SPb = [ar.alloc([512], BF16) for _ in range(NEB)]
        X = [ar.alloc([512], F32) for _ in range(3)]
        A = [ar.alloc([512], BF16) for _ in range(3)]
        m3 = ar.mark()
        blks = []
        for g in range(NG):
            kbs = list(range(4 * g + 3, -1, -1))
            for hp_ in range(0, 8, 2):
                for ii, kb in enumerate(kbs):
                    for c in range(2):
                        blks.append(dict(h=hp_ + c, g=g, kb=kb, first=(ii == 0), last=(ii == len(kbs) - 1), grp=c))
        nb_ = len(blks)

        def geo(bk):
            jl = max(0, bk["kb"] - 4 * bk["g"])
            return jl * 128, 512 - jl * 128, bk["kb"] >= 4 * bk["g"]

        def sZ(n):
            bk = blks[n]
            q0, N, diag = geo(bk)
            h, g, kb = bk["h"], bk["g"], bk["kb"]
            bz = n % 3
            P.pe(I("matmul", out=PS[bz][:, 0:N], lhsT=BK[:, h, kb * 128:(kb + 1) * 128], rhs=BQ[:, h, g * 512 + q0:(g + 1) * 512], start=True, stop=True),
                 r=[("BK", h), ("BQ", h)], w=[psk(bz)])

        def sE(n):
            bk = blks[n]
            q0, N, diag = geo(bk)
            bz = n % 3
            e_, sp_ = E[n % NEB], SPb[n % NEB]
            ke, ksp = ("E", n % NEB), ("SP", n % NEB)
            P.act(I("activation", out=e_[:, 0:N], in_=PS[bz][:, 0:N], func=AF.Exp, scale=QK_SCALE), w=[psk(bz), ke])
            P.act(I("activation", out=sp_[:, 0:N], in_=e_[:, 0:N], func=AF.Ln, bias=onesF[:, 0:1], scale=1.0), r=[ke, "onesF"], w=[ksp])
            if diag:
                P.pool(I("tensor_tensor", out=sp_[:, 0:128], in0=sp_[:, 0:128], in1=causF, op=ALU.mult), r=["causF"], w=[ksp])

        def sRa(n):
            bk = blks[n]
            q0, N, diag = geo(bk)
            br = 3 + bk["grp"]
            P.pe(I("matmul", out=PS[br][:, q0:512], lhsT=triB, rhs=SPb[n % NEB][:, 0:N], start=bk["first"], stop=False, skip_group_check=True),
                 r=[("SP", n % NEB), "triB"], w=[psk(br)])

        def sX(n):
            bk = blks[n]
            q0, N, diag = geo(bk)
            br = 3 + bk["grp"]
            P.act(I("activation", out=X[n % 3][:, 0:N], in_=PS[br][:, q0:512], func=AF.Exp, scale=-1.0), w=[psk(br), ("X", n % 3)])

        def sRbA(n):
            bk = blks[n]
            q0, N, diag = geo(bk)
            br = 3 + bk["grp"]
            if not bk["last"]:
                P.pe(I("matmul", out=PS[br][:, q0:512], lhsT=u2B, rhs=SPb[n % NEB][:, 0:N], start=False, stop=False, skip_group_check=True),
                     r=[("SP", n % NEB), "u2B"], w=[psk(br)])
            a_ = A[n % 3]
            P.dve(I("tensor_tensor", out=a_[:, 0:N], in0=E[n % NEB][:, 0:N], in1=X[n % 3][:, 0:N], op=ALU.mult), r=[("E", n % NEB), ("X", n % 3)], w=[("A", n % 3)])
            if diag:
                P.pool(I("tensor_tensor", out=a_[:, 0:128], in0=a_[:, 0:128], in1=causF, op=ALU.mult), r=["causF"], w=[("A", n % 3)])

        def sO(n):
            bk = blks[n]
            q0, N, diag = geo(bk)
            h, g, kb = bk["h"], bk["g"], bk["kb"]
            bo = 5 + bk["grp"]
            P.pe(I("matmul", out=PS[bo][:, q0:512], lhsT=BV[:, kb, h * 128:(h + 1) * 128], rhs=A[n % 3][:, 0:N], start=bk["first"], stop=bk["last"], skip_group_check=True),
                 r=[("A", n % 3), ("BV", kb)], w=[psk(bo)])
            if bk["last"]:
                if bk["grp"] == 0:
                    P.act(I("copy", out=BA[:, h, g * 512:(g + 1) * 512], in_=PS[bo]), w=[psk(bo), "oT"])
                else:
                    P.dve(I("tensor_copy", out=BA[:, h, g * 512:(g + 1) * 512], in_=PS[bo]), w=[psk(bo), "oT"])

        for n in range(nb_ + 5):
            if n < nb_:
                sZ(n)
            if 0 <= n - 4 < nb_:
                sRbA(n - 4)
            if 0 <= n - 1 < nb_:
                sE(n - 1)
            if 0 <= n - 2 < nb_:
                sRa(n - 2)
            if 0 <= n - 3 < nb_:
                sX(n - 3)
            if 0 <= n - 5 < nb_:
                sO(n - 5)
        P.barrier()
        ar.release(m2)
        P.dma(I("dma_start", out=WBIG, in_=b_w_out.rearrange("(k p) c -> p k c", p=128)), w=["WBIG"], q="pool")
        gb = ar.alloc([2, D], F32)
        load_gb(gb, 2)
        xs2 = [ar.alloc([D], F32) for _ in range(3)]
        pres = [ar.alloc([D], F32) for _ in range(5)]
        smalls = [ln_smalls() for _ in range(5)]
        out_proj_ln(BA, WBIG, h2, h3, tok0, 2, xs2, pres, gb, smalls)
        P.barrier()

    def moe(layer, src, dst, lnidx):
        SG = S
        NTS = SG // 128
        for sgi in range(NT // SG):
            tok0 = sgi * SG
            ar.release(base_mark)
            hTb = ar.alloc([8, SG], BF16)
            ysb = ar.alloc([NTS, D], F32)
            gates = ar.alloc([NTS, NE], F32)
            Wg = [ar.alloc([8, DFF], BF16) for _ in range(2)]
            Wu = [ar.alloc([8, DFF], BF16) for _ in range(2)]
            Wd = [ar.alloc([2, D], BF16) for _ in range(2)]
            gb = ar.alloc([2, D], F32)
            load_gb(gb, lnidx)
            m2 = ar.mark()
            hs = [ar.alloc([D], F32) for _ in range(3)]
            hT32 = [ar.alloc([8, 128], F32) for _ in range(3)]
            sgt = [ar.alloc([512], F32) for _ in range(2)]
            he = [ar.alloc([2, 512], BF16) for _ in range(3)]
            stg = [ar.alloc([8, DFF], F32), ar.alloc([8, DFF], F32), ar.alloc([2, D], F32)]
            lg_all = ar.alloc([NTS, NE], F32)
            ex = ar.alloc([NTS, NE], F32)
            pr = ar.alloc([NTS, NE], F32)
            sel = ar.alloc([NTS, NE], F32)
            gt = ar.alloc([NTS, NE], F32)
            mx = ar.alloc([NTS], F32)
            se = ar.alloc([NTS], F32)
            q4 = [ar.alloc([NTS, 4], F32) for _ in range(8)]
            bst_all = ar.alloc([NTS, 2, 6], F32)
            mv_all = ar.alloc([NTS, 2], F32)
            rstd_all = ar.alloc([NTS], F32)
            nmr_all = ar.alloc([NTS], F32)
            def r0(i):
                hb, kh = hs[i % 3], ("hs", i % 3)
                h32, k32 = hT32[i % 3], ("hT32", i % 3)
                P.dma(I("dma_start", out=hb, in_=src[tok0 + i * 128: tok0 + (i + 1) * 128, :]), w=[kh])
                ba, bb = (0, 1) if i % 2 == 0 else (2, 3)
                calls = []
                for k in range(8):
                    bank = ba if k < 4 else bb
                    calls.append(("transpose", dict(out=PS[bank][:, (k % 4) * 128:(k % 4 + 1) * 128], in_=hb[:, k * 128:(k + 1) * 128], identity=identF)))
                P.pe(G(calls), r=[kh, "identF"], w=[psk(ba), psk(bb)])
                P.act(I("copy", out=h32[:, 0:4, :], in_=PS[ba].rearrange("p (a b) -> p a b", a=4)), w=[psk(ba), (k32, 0)])
                P.dve(I("tensor_copy", out=h32[:, 4:8, :], in_=PS[bb].rearrange("p (a b) -> p a b", a=4)), w=[psk(bb), (k32, 1)])

            def r1(i):
                hb, kh = hs[i % 3], ("hs", i % 3)
                h32, k32 = hT32[i % 3], ("hT32", i % 3)
                P.pool(I("tensor_copy", out=hTb[:, :, i * 128:(i + 1) * 128], in_=h32), r=[(k32, 0), (k32, 1)], w=[("hTb", i)])
                P.pool(I("tensor_scalar", out=ysb[:, i, :], in0=hb, scalar1=ALPHA, scalar2=None, op0=ALU.mult), r=[kh], w=[("ysb", i)])
                bk = 4 + i % 2
                P.pe(G([MM(PS[bk][:, 0:NE], h32[:, k, :], rw32[:, k, :], start=(k == 0), stop=(k == 7)) for k in range(8)]), r=[(k32, 0), (k32, 1), "rw32"], w=[psk(bk)])
                P.dve(I("tensor_copy", out=lg_all[:, i, :], in_=PS[bk][:, 0:NE]), w=[psk(bk), ("lg", i)])
            for n in range(NTS + 1):
                if n < NTS:
                    r0(n)
                if n >= 1:
                    r1(n - 1)
            kg = "gat"
            lgk = [("lg", i) for i in range(NTS)]
            B3 = [128, NTS, NE]
            B4 = [128, NTS, 4, 4]

            def v4(t):
                return t.rearrange("p n (a b) -> p n a b", a=4)
            P.dve(I("tensor_reduce", out=mx, in_=lg_all, axis=AX.X, op=ALU.max), r=lgk, w=[kg])
            P.dve(I("tensor_tensor", out=ex, in0=lg_all, in1=mx.unsqueeze(2).to_broadcast(B3), op=ALU.subtract), r=lgk, w=[kg])
            P.act(I("activation", out=ex, in_=ex, func=AF.Exp), w=[kg])
            P.dve(I("tensor_reduce", out=se, in_=ex, axis=AX.X, op=ALU.add), w=[kg])
            P.dve(I("reciprocal", out=se, in_=se), w=[kg])
            P.dve(I("tensor_tensor", out=pr, in0=ex, in1=se.unsqueeze(2).to_broadcast(B3), op=ALU.mult), w=[kg])
            P.dve(I("tensor_tensor", out=sel, in0=pr, in1=rbias.unsqueeze(1).to_broadcast(B3), op=ALU.add), r=["rbias"], w=[kg])
            s4 = v4(sel)
            a_, b_, c_, d_ = s4[:, :, :, 0], s4[:, :, :, 1], s4[:, :, :, 2], s4[:, :, :, 3]
            P.dve(I("tensor_tensor", out=q4[0], in0=a_, in1=b_, op=ALU.max), w=[kg])
            P.dve(I("tensor_tensor", out=q4[1], in0=a_, in1=b_, op=ALU.min), w=[kg])
            P.dve(I("tensor_tensor", out=q4[2], in0=c_, in1=d_, op=ALU.max), w=[kg])
            P.dve(I("tensor_tensor", out=q4[3], in0=c_, in1=d_, op=ALU.min), w=[kg])
            P.dve(I("tensor_tensor", out=q4[4], in0=q4[0], in1=q4[2], op=ALU.max), w=[kg])
            P.dve(I("tensor_tensor", out=q4[5], in0=q4[0], in1=q4[2], op=ALU.min), w=[kg])
            P.dve(I("tensor_tensor", out=q4[6], in0=q4[1], in1=q4[3], op=ALU.max), w=[kg])
            P.dve(I("tensor_tensor", out=q4[7], in0=q4[5], in1=q4[6], op=ALU.max), w=[kg])
            P.dve(I("tensor_tensor", out=q4[0], in0=q4[4], in1=q4[7], op=ALU.add), w=[kg])
            P.dve(I("tensor_reduce", out=mx, in_=q4[0], axis=AX.X, op=ALU.max), w=[kg])
            P.dve(I("tensor_tensor", out=q4[1], in0=q4[0], in1=mx.unsqueeze(2).to_broadcast([128, NTS, 4]), op=ALU.is_ge), w=[kg])
            P.dve(I("tensor_tensor", out=v4(gt), in0=s4, in1=q4[7].unsqueeze(3).to_broadcast(B4), op=ALU.is_ge), w=[kg])
            P.dve(I("tensor_tensor", out=v4(gt), in0=v4(gt), in1=q4[1].unsqueeze(3).to_broadcast(B4), op=ALU.mult), w=[kg])
            P.dve(I("tensor_tensor", out=gt, in0=gt, in1=pr, op=ALU.mult), w=[kg])
            P.dve(I("tensor_reduce", out=se, in_=gt, axis=AX.X, op=ALU.add), w=[kg])
            P.dve(I("reciprocal", out=se, in_=se), w=[kg])
            P.dve(I("tensor_tensor", out=gates, in0=gt, in1=se.unsqueeze(2).to_broadcast(B3), op=ALU.mult), w=[kg, "gates"])
            steps = [(e, tg) for e in range(NE) for tg in range(SG // 512)]
            nY = [0]

            def load_w(e):
                kw = ("Wexp", e % 2)
                P.dma(I("dma_start", out=stg[0], in_=w_gate[layer, e].rearrange("(k p) f -> p k f", p=128)), w=[("stg", 0)])
                P.pool(I("tensor_copy", out=Wg[e % 2], in_=stg[0]), r=[("stg", 0)], w=[kw])
                P.dma(I("dma_start", out=stg[1], in_=w_up[layer, e].rearrange("(k p) f -> p k f", p=128)), w=[("stg", 1)])
                P.pool(I("tensor_copy", out=Wu[e % 2], in_=stg[1]), r=[("stg", 1)], w=[kw])
                P.dma(I("dma_start", out=stg[2], in_=w_down[layer, e].rearrange("(c p) d -> p c d", p=128)), w=[("stg", 2)])
                P.pool(I("tensor_copy", out=Wd[e % 2], in_=stg[2]), r=[("stg", 2)], w=[kw])

            def gu(n):
                e, tg = steps[n]
                wg, wu = Wg[e % 2], Wu[e % 2]
                kw = ("Wexp", e % 2)
                c0, c1 = tg * 512, (tg + 1) * 512
                heb = he[n % 3]
                khe = ("he", n % 3)
                hk = [("hTb", t) for t in range(tg * 4, tg * 4 + 4)]
                for fc in range(2):
                    pg, pu = 2 * fc, 2 * fc + 1
                    P.pe(G([MM(PS[pg], wg[:, k, fc * 128:(fc + 1) * 128], hTb[:, k, c0:c1], start=(k == 0), stop=(k == 7)) for k in range(8)] +
                           [MM(PS[pu], wu[:, k, fc * 128:(fc + 1) * 128], hTb[:, k, c0:c1], start=(k == 0), stop=(k == 7)) for k in range(8)]),
                         r=[kw] + hk, w=[psk(pg), psk(pu)])
                for fc in range(2):
                    pg, pu = 2 * fc, 2 * fc + 1
                    st_ = sgt[fc]
                    kst = ("sgt", fc)
                    P.act(I("activation", out=st_, in_=PS[pg], func=AF.Silu), w=[psk(pg), kst])
                    P.dve(I("tensor_tensor", out=heb[:, fc, :], in0=st_, in1=PS[pu], op=ALU.mult), r=[kst], w=[psk(pu), khe])

            def down(n):
                e, tg = steps[n]
                wd = Wd[e % 2]
                kw = ("Wexp", e % 2)
                heb = he[n % 3]
                khe = ("he", n % 3)
                for jt in range(4):
                    i = tg * 4 + jt
                    for c in range(2):
                        by = 4 + nY[0] % 4
                        nY[0] += 1
                        P.pe(G([MM(PS[by], heb[:, fc, jt * 128:(jt + 1) * 128], wd[:, fc, c * 512:(c + 1) * 512], start=(fc == 0), stop=(fc == 1)) for fc in range(2)]),
                             r=[khe, kw], w=[psk(by)])
                        P.dve(I("scalar_tensor_tensor", out=ysb[:, i, c * 512:(c + 1) * 512], in0=PS[by], scalar=gates[:, i, e:e + 1], in1=ysb[:, i, c * 512:(c + 1) * 512], op0=ALU.mult, op1=ALU.add),
                              r=["gates"], w=[psk(by), ("ysb", i)])
                if tg == SG // 512 - 1 and e + 2 < NE:
                    load_w(e + 2)
            load_w(0)
            load_w(1)
            gu(0)
            for n in range(len(steps)):
                if n + 1 < len(steps):
                    gu(n + 1)
                down(n)
            for i in range(NTS):
                P.dve(I("bn_stats", out=bst_all[:, i, 0, :], in_=ysb[:, i, 0:512]), r=[("ysb", i)], w=[("bst", i, 0)])
                P.dve(I("bn_stats", out=bst_all[:, i, 1, :], in_=ysb[:, i, 512:1024]), r=[("ysb", i)], w=[("bst", i, 1)])
                P.dve(I("bn_aggr", out=mv_all[:, i, :], in_=bst_all[:, i]), r=[("bst", i, 0), ("bst", i, 1)], w=[("mv", i)])
            mvk = [("mv", i) for i in range(NTS)]
            P.act(I("activation", out=rstd_all, in_=mv_all[:, :, 1], func=AF.Ln, bias=epsT[:, 0:1], scale=1.0), r=mvk + ["epsT"], w=["rstd"])
            P.act(I("activation", out=rstd_all, in_=rstd_all, func=AF.Exp, scale=-0.5), w=["rstd"])
            P.dve(I("scalar_tensor_tensor", out=nmr_all, in0=mv_all[:, :, 0], scalar=-1.0, in1=rstd_all, op0=ALU.mult, op1=ALU.mult), r=mvk + ["rstd"], w=["nmr"])
            for i in range(NTS):
                kp = ("ysb", i)
                yi = ysb[:, i, :]
                P.act(I("activation", out=yi, in_=yi, func=AF.Identity, scale=rstd_all[:, i:i + 1], bias=nmr_all[:, i:i + 1]), r=["rstd", "nmr"], w=[kp])
                P.pool(I("tensor_tensor", out=yi, in0=yi, in1=gb[:, 0, :], op=ALU.mult), r=["gb"], w=[kp])
                P.pool(I("tensor_tensor", out=yi, in0=yi, in1=gb[:, 1, :], op=ALU.add), r=["gb"], w=[kp])
                P.dma(I("dma_start", out=dst[tok0 + i * 128: tok0 + (i + 1) * 128, :], in_=yi), r=[kp], w=[("dst", i)])
            P.barrier()

    stages = [("A", lambda: [mixer_A(b) for b in range(NSEQ)]),
              ("M0", lambda: moe(0, h1, h2, 1)),
              ("B", lambda: [mixer_B(b) for b in range(NSEQ)]),
              ("M1", lambda: moe(1, h3, out, 3))]
    try:
        for name, fn in stages:
            fn()
            if stop_after == name:
                break
    except _Stop:
        pass
    P.emit(nc)
    es.close()
    nc._prog_stats = {e: len(P.eng_ops[e]) for e in ENGS}
    return nc


def rope_tables(S):
    def tab(dim, rep):
        inv = (1.0 / (10000.0 ** (np.arange(0, dim, 2, dtype=np.float32) / np.float32(dim)))).astype(np.float32)
        ang = (np.arange(S, dtype=np.float32)[:, None] * inv[None, :]).astype(np.float32)
        c = np.cos(ang).astype(np.float32).T
        s = np.sin(ang).astype(np.float32).T
        cosT = np.concatenate([c, c], 0)
        sinT = np.concatenate([-s, s], 0)
        return np.tile(cosT, (rep, 1)), np.tile(sinT, (rep, 1))
    c128, s128 = tab(128, 1)
    c64, s64 = tab(64, 2)
    return np.ascontiguousarray(np.stack([c128, s128, c64, s64], 0).astype(np.float32))


def host_consts(S):
    ii = np.arange(128)
    return {
        "c_ident": np.eye(128, dtype=np.float32),
        "c_tri": (ii[:, None] >= ii[None, :]).astype(np.float32),
        "c_caus": (ii[:, None] < ii[None, :]).astype(np.float32),
        "c_rope": rope_tables(S),
    }


def make_in_maps(inputs, S, NSEQ, ncores):
    x = np.asarray(inputs["x"], dtype=np.float32)
    cols = a_ext_cols()
    shared = {
        "w_in_ext": np.ascontiguousarray(np.asarray(inputs["a_w_in"], np.float32)[0][:, cols]),
        "a_w_out": np.ascontiguousarray(np.asarray(inputs["a_w_out"], np.float32)[0]),
        "b_w_q": np.ascontiguousarray(np.asarray(inputs["b_w_q"], np.float32)[0]),
        "b_w_kv": np.ascontiguousarray(np.asarray(inputs["b_w_kv"], np.float32)),
        "b_w_out": np.ascontiguousarray(np.asarray(inputs["b_w_out"], np.float32)[0]),
        "router_w": np.ascontiguousarray(np.asarray(inputs["router_w"], np.float32)),
        "router_bias": np.ascontiguousarray(np.asarray(inputs["router_bias"], np.float32).reshape(1, NE)),
        "exp_w_gate": np.ascontiguousarray(np.asarray(inputs["exp_w_gate"], np.float32)),
        "exp_w_up": np.ascontiguousarray(np.asarray(inputs["exp_w_up"], np.float32)),
        "exp_w_down": np.ascontiguousarray(np.asarray(inputs["exp_w_down"], np.float32)),
        "ln_g": np.ascontiguousarray(np.asarray(inputs["ln_g"], np.float32).reshape(4, D)),
        "ln_b": np.ascontiguousarray(np.asarray(inputs["ln_b"], np.float32).reshape(4, D)),
    }
    shared.update(host_consts(S))
    maps = []
    for c in range(ncores):
        m = dict(shared)
        m["x"] = np.ascontiguousarray(x[c * NSEQ:(c + 1) * NSEQ].reshape(NSEQ * S, D))
        maps.append(m)
    return maps


_NC_CACHE = {}


def kernel(**inputs):
    x = np.asarray(inputs["x"])
    B, S, _ = x.shape
    ncores = 8
    NSEQ = B // ncores
    key = (S, NSEQ)
    if key not in _NC_CACHE:
        _NC_CACHE[key] = build(S, NSEQ, min(256, S // 4))
    nc = _NC_CACHE[key]
    maps = make_in_maps(inputs, S, NSEQ, ncores)
    res = run_bass_kernel_spmd(nc, maps, core_ids=list(range(ncores)))
    outs = [np.asarray(r["out"], dtype=np.float32).reshape(NSEQ, S, D) for r in res.results]
    return np.concatenate(outs, axis=0)
```

```python
import math
from contextlib import ExitStack

import numpy as np

import concourse.bass as bass
import concourse.mybir as mybir
from concourse.bass_utils import run_bass_kernel_spmd

F32 = mybir.dt.float32
BF16 = mybir.dt.bfloat16
AF = mybir.ActivationFunctionType
ALU = mybir.AluOpType
AX = mybir.AxisListType

D = 1024
NH = 8
HD = 128
NE = 16
DFF = 256
ALPHA = 4.0 ** 0.25
LN_EPS = 1e-5
NEG = -30000.0
QK_SCALE = HD ** -0.5
IDX_C = (8 ** -0.5) * (64 ** -0.5)
NIT = 18

ENGS = ("pe", "act", "dve", "pool", "sp")
NRING = 12


class _Op:
    __slots__ = ("id", "eng", "fn", "deps", "is_dma", "ring", "ringval", "marked", "val", "pos")

    def __init__(self, id, eng, fn, is_dma):
        self.id = id
        self.eng = eng
        self.fn = fn
        self.deps = set()
        self.is_dma = is_dma
        self.ring = None
        self.ringval = 0
        self.marked = False
        self.val = 0
        self.pos = 0


class Prog:
    def __init__(self):
        self.ops = []
        self.lastw = {}
        self.readers = {}
        self.eng_ops = {e: [] for e in ENGS}
        self.ring_cnt = {}
        self.ring_last = {}
        self.dma_n = {e: 0 for e in ENGS}
        self._bar_pending = {}
        self._bar_set = ()

    def _add(self, eng, fn, r, w, is_dma=False):
        op = _Op(len(self.ops), eng, fn, is_dma)
        deps = set()
        if self._bar_pending.get(eng):
            self._bar_pending[eng] = False
            deps |= self._bar_set
        for k in r:
            if k in self.lastw:
                deps.add(self.lastw[k])
        for k in w:
            if k in self.lastw:
                deps.add(self.lastw[k])
            deps |= self.readers.get(k, set())
        for k in r:
            self.readers.setdefault(k, set()).add(op.id)
        for k in w:
            self.lastw[k] = op.id
            self.readers[k] = set()
        if is_dma:
            slot = (eng, self.dma_n[eng] % NRING)
            self.dma_n[eng] += 1
            op.ring = slot
            self.ring_cnt[slot] = self.ring_cnt.get(slot, 0) + 1
            op.ringval = 16 * self.ring_cnt[slot]
            if slot in self.ring_last:
                deps.add(self.ring_last[slot])
            self.ring_last[slot] = op.id
        deps.discard(op.id)
        op.deps = deps
        op.pos = len(self.eng_ops[eng])
        self.eng_ops[eng].append(op)
        self.ops.append(op)
        return op

    def barrier(self):
        s = set()
        for e in ENGS:
            if self.eng_ops[e]:
                s.add(self.eng_ops[e][-1].id)
        for slot, oid in self.ring_last.items():
            s.add(oid)
        self._bar_set = s
        self._bar_pending = {e: True for e in ENGS}
        self.lastw = {}
        self.readers = {}

    def pe(self, fn, r=(), w=()):
        return self._add("pe", fn, r, w)

    def act(self, fn, r=(), w=()):
        return self._add("act", fn, r, w)

    def dve(self, fn, r=(), w=()):
        return self._add("dve", fn, r, w)

    def pool(self, fn, r=(), w=()):
        return self._add("pool", fn, r, w)

    def dma(self, fn, r=(), w=(), q="sp"):
        return self._add(q, fn, r, w, is_dma=True)

    def emit(self, nc):
        ops = self.ops
        fin = set()
        for e in ENGS:
            if self.eng_ops[e]:
                fin.add(self.eng_ops[e][-1].id)
        for slot, oid in self.ring_last.items():
            fin.add(oid)
        for op in ops:
            nd = set()
            for d in op.deps:
                dop = ops[d]
                if dop.eng == op.eng and not dop.is_dma and not op.is_dma:
                    if op.eng == "pe":
                        continue
                    if op.pos - dop.pos > 1:
                        continue
                nd.add(d)
            op.deps = nd
        for op in ops:
            for d in op.deps:
                ops[d].marked = True
        for d in fin:
            ops[d].marked = True
        for e in ENGS:
            c = 0
            for op in self.eng_ops[e]:
                if op.is_dma:
                    continue
                if op.marked:
                    c += 1
                    op.val = c
        with ExitStack() as st:
            sems = {e: st.enter_context(nc.semaphore("s_" + e)) for e in ENGS}
            rings = {}
            for slot in self.ring_cnt:
                rings[slot] = st.enter_context(nc.semaphore("r_%s_%d" % slot))
            block = st.enter_context(nc.Block())

            def token(op):
                if op.is_dma:
                    return (rings[op.ring], op.ringval, ("r",) + op.ring)
                return (sems[op.eng], op.val, ("e", op.eng))

            def run(e, eng):
                waited = {}
                for op in self.eng_ops[e]:
                    need = {}
                    for d in op.deps:
                        s, v, key = token(ops[d])
                        if waited.get(key, 0) >= v:
                            continue
                        if need.get(key, (None, 0))[1] < v:
                            need[key] = (s, v)
                    for key, (s, v) in need.items():
                        eng.wait_ge(s, v)
                        waited[key] = v
                    ins = op.fn(eng)
                    if op.is_dma:
                        ins.then_inc(rings[op.ring], 16)
                    elif op.marked:
                        ins.then_inc(sems[e], 1)
                if e == "sp":
                    for d in sorted(fin):
                        s, v, key = token(ops[d])
                        if waited.get(key, 0) >= v:
                            continue
                        eng.wait_ge(s, v)
                        waited[key] = v

            block.tensor(lambda eng: run("pe", eng))
            block.scalar(lambda eng: run("act", eng))
            block.vector(lambda eng: run("dve", eng))
            block.gpsimd(lambda eng: run("pool", eng))
            block.sync(lambda eng: run("sp", eng))


def I(name, *a, **kw):
    return lambda e: getattr(e, name)(*a, **kw)


def G(calls):
    calls = list(calls)

    def fn(e):
        ins = None
        for (name, kw) in calls:
            ins = getattr(e, name)(**kw)
        return ins
    return fn


def MM(out, lhsT, rhs, start=True, stop=True, skip=False):
    kw = dict(out=out, lhsT=lhsT, rhs=rhs, start=start, stop=stop)
    if skip:
        kw["skip_group_check"] = True
    return ("matmul", kw)


class _Stop(Exception):
    pass


class Arena:
    def __init__(self, ap, nwords):
        self.ar = ap
        self.n = nwords
        self.top = 0

    def mark(self):
        return self.top

    def release(self, m):
        self.top = m

    def alloc(self, shape, dt):
        nelem = 1
        for s in shape:
            nelem *= s
        nb = 2 if dt == BF16 else 4
        words = (nelem * nb + 3) // 4
        off = self.top
        self.top += words
        assert self.top <= self.n, "arena overflow %d > %d" % (self.top, self.n)
        v = self.ar[:, off:off + words]
        if dt != F32:
            v = v.bitcast(dt)[:, 0:nelem]
        if len(shape) == 2:
            v = v.rearrange("p (a b) -> p a b", a=shape[0])
        elif len(shape) == 3:
            v = v.rearrange("p (a b c) -> p a b c", a=shape[0], b=shape[1])
        return v


def a_ext_cols():
    cols = []
    q0, k0, v0, qi0, ki0, wi0 = 0, 1024, 2048, 3072, 3584, 3648

    def perm(base, width):
        half = width // 2
        return [base + (i + half) % width for i in range(width)]
    for h in range(8):
        cols += list(range(q0 + h * 128, q0 + (h + 1) * 128))
    for h in range(8):
        cols += perm(q0 + h * 128, 128)
    for h in range(8):
        cols += list(range(k0 + h * 128, k0 + (h + 1) * 128))
    for h in range(8):
        cols += perm(k0 + h * 128, 128)
    for h in range(8):
        cols += list(range(qi0 + h * 64, qi0 + (h + 1) * 64))
    for h in range(8):
        cols += perm(qi0 + h * 64, 64)
    cols += list(range(ki0, ki0 + 64)) * 2
    cols += perm(ki0, 64) * 2
    cols += list(range(v0, v0 + 1024))
    cols += list(range(wi0, wi0 + 8))
    return np.array(cols, dtype=np.int64)


BLK_Q, BLK_QP, BLK_K, BLK_KP, BLK_QI, BLK_QIP, BLK_KI, BLK_KIP = 0, 8, 16, 24, 32, 36, 40, 41
COL_V = 42 * 128
COL_WI = COL_V + 1024
NCOL_EXT = COL_WI + 8


def build(S, NSEQ, TOPK, stop_after=None, debug=False):
    NT = NSEQ * S
    NTL = S // 128
    NG = S // 512
    assert S % 512 == 0
    nc = bass.Bass("TRN2", target_bir_lowering=False)

    def din(name, shape):
        return nc.dram_tensor(name, shape, F32, kind="ExternalInput").ap()
    x = din("x", [NT, D])
    w_in = din("w_in_ext", [D, NCOL_EXT])
    a_w_out = din("a_w_out", [D, D])
    b_w_q = din("b_w_q", [D, D])
    b_w_kv = din("b_w_kv", [D, 2 * D])
    b_w_out = din("b_w_out", [D, D])
    router_w = din("router_w", [D, NE])
    router_b = din("router_bias", [1, NE])
    w_gate = din("exp_w_gate", [2, NE, D, DFF])
    w_up = din("exp_w_up", [2, NE, D, DFF])
    w_down = din("exp_w_down", [2, NE, DFF, D])
    ln_g = din("ln_g", [4, D])
    ln_b = din("ln_b", [4, D])
    c_ident = din("c_ident", [128, 128])
    c_tri = din("c_tri", [128, 128])
    c_caus = din("c_caus", [128, 128])
    c_rope = din("c_rope", [4, 128, S])
    out = nc.dram_tensor("out", [NT, D], F32, kind="ExternalOutput").ap()
    skind = "ExternalOutput" if debug else "Internal"
    h1 = nc.dram_tensor("h1", [NT, D], F32, kind=skind).ap()
    h2 = nc.dram_tensor("h2", [NT, D], F32, kind=skind).ap()
    h3 = nc.dram_tensor("h3", [NT, D], F32, kind=skind).ap()
    qT_d = nc.dram_tensor("qT_d", [8, 128, S], BF16, kind="Internal").ap()
    qiT_d = nc.dram_tensor("qiT_d", [4, 128, S], BF16, kind="Internal").ap()

    P = Prog()
    es = ExitStack()
    ARW = 51800
    arena_t = es.enter_context(nc.sbuf_tensor("arena", [128, ARW], F32))
    ar = Arena(arena_t, ARW)
    psum_t = es.enter_context(nc.psum_tensor("psum", [128, 8, 512], F32))
    PS = [psum_t[:, b, :] for b in range(8)]
    PSB = [psum_t[:, b, :].bitcast(BF16) for b in range(8)]

    def psk(b):
        return ("ps", b)

    identF = ar.alloc([128], F32)
    identB = ar.alloc([128], BF16)
    onesB = ar.alloc([128], BF16)
    onesF = ar.alloc([128], F32)
    triB = ar.alloc([128], BF16)
    causF = ar.alloc([128], F32)
    u2B = ar.alloc([128], BF16)
    ctmp = ar.alloc([128], F32)
    rw32 = ar.alloc([8, NE], F32)
    rbias = ar.alloc([NE], F32)
    pw = ar.alloc([NIT], F32)
    sel8 = ar.alloc([4, 8], F32)
    epsT = ar.alloc([1], F32)
    P.dma(I("dma_start", out=identF, in_=c_ident), w=["identF"])
    P.dve(I("tensor_copy", out=identB, in_=identF), r=["identF"], w=["identB"])
    P.dve(I("memset", onesB, 1.0), w=["onesB"])
    P.dve(I("memset", onesF, 1.0), w=["onesF"])
    P.dma(I("dma_start", out=ctmp, in_=c_tri), w=["ctmp"])
    P.dve(I("tensor_copy", out=triB, in_=ctmp), r=["ctmp"], w=["triB"])
    P.dma(I("dma_start", out=causF, in_=c_caus), w=["causF"])
    P.dve(I("tensor_copy", out=u2B, in_=causF), r=["causF"], w=["u2B"])
    P.dma(I("dma_start", out=rw32, in_=router_w.rearrange("(k p) e -> p k e", p=128)), w=["rw32"])
    P.dma(I("dma_start", out=rbias, in_=router_b.broadcast_to([128, NE])), w=["rbias"])
    for it in range(NIT):
        P.dve(I("memset", pw[:, it:it + 1], 2.0 ** -(it + 1)), w=["pw"])
    P.dve(I("memset", sel8, -1e30), w=["sel8"])
    P.dve(I("memset", epsT, LN_EPS), w=["epsT"])
    base_mark = ar.mark()
    P.barrier()

    def layer_norm(pre, kpre, yo, kyo, gb, small, kidx):
        bst, mv, lnv, rstd, nmr = small
        ks = ("lnsm", kidx)
        P.dve(I("bn_stats", out=bst[:, 0, :], in_=pre[:, 0:512]), r=[kpre], w=[ks])
        P.dve(I("bn_stats", out=bst[:, 1, :], in_=pre[:, 512:1024]), r=[kpre], w=[ks])
        P.dve(I("bn_aggr", out=mv, in_=bst), r=[ks], w=[ks])
        P.act(I("activation", out=lnv, in_=mv[:, 1:2], func=AF.Ln, bias=epsT[:, 0:1], scale=1.0), r=[ks, "epsT"], w=[ks])
        P.act(I("activation", out=rstd, in_=lnv, func=AF.Exp, scale=-0.5), r=[ks], w=[ks])
        P.dve(I("tensor_scalar", out=nmr, in0=mv[:, 0:1], scalar1=rstd[:, 0:1], scalar2=-1.0, op0=ALU.mult, op1=ALU.mult), r=[ks], w=[ks])
        P.act(I("activation", out=pre, in_=pre, func=AF.Identity, scale=rstd[:, 0:1], bias=nmr[:, 0:1]), r=[ks], w=[kpre])
        P.pool(I("tensor_tensor", out=pre, in0=pre, in1=gb[:, 0, :], op=ALU.mult), r=["gb"], w=[kpre])
        P.pool(I("tensor_tensor", out=yo, in0=pre, in1=gb[:, 1, :], op=ALU.add), r=["gb", kpre], w=[kyo])

    def ln_smalls():
        return (ar.alloc([2, 6], F32), ar.alloc([2], F32), ar.alloc([1], F32), ar.alloc([1], F32), ar.alloc([1], F32))

    def load_gb(gb, idx):
        P.dma(I("dma_start", out=gb[:, 0, :], in_=ln_g[idx:idx + 1, :].broadcast_to([128, D])), w=["gb"])
        P.dma(I("dma_start", out=gb[:, 1, :], in_=ln_b[idx:idx + 1, :].broadcast_to([128, D])), w=["gb"])

    def build_xT(src, tok0, BA, xs):
        for i in range(NTL):
            xb = xs[i % len(xs)]
            kx = ("xs", i % len(xs))
            P.dma(I("dma_start", out=xb, in_=src[tok0 + i * 128: tok0 + (i + 1) * 128, :]), w=[kx])
            ba, bb = (0, 1) if i % 2 == 0 else (2, 3)
            calls = []
            for k in range(8):
                bank = ba if k < 4 else bb
                calls.append(("transpose", dict(out=PS[bank][:, (k % 4) * 128:(k % 4 + 1) * 128], in_=xb[:, k * 128:(k + 1) * 128], identity=identF)))
            P.pe(G(calls), r=[kx, "identF"], w=[psk(ba), psk(bb)])
            P.act(I("copy", out=BA[:, 0:4, i * 128:(i + 1) * 128], in_=PS[ba].rearrange("p (a b) -> p a b", a=4)), w=[psk(ba), ("BA", i, 0)])
            P.dve(I("tensor_copy", out=BA[:, 4:8, i * 128:(i + 1) * 128], in_=PS[bb].rearrange("p (a b) -> p a b", a=4)), w=[psk(bb), ("BA", i, 1)])

    def ba_keys(t0, t1):
        ks = []
        for i in range(t0 // 128, (t1 + 127) // 128):
            ks += [("BA", i, 0), ("BA", i, 1)]
        return ks

    def load_wcol(dst, key, wap, c0):
        P.dma(I("dma_start", out=dst, in_=wap[:, c0:c0 + 128].rearrange("(k p) c -> p k c", p=128)), w=[key], q="pool")

    def out_proj_ln(BA, WBIG, src, dst, tok0, lnidx, xs, pres, gb, smalls):
        NX, NP_ = len(xs), len(pres)

        def s0(i):
            xb, kx = xs[i % NX], ("xs", i % NX)
            pre, kp = pres[i % NP_], ("pre", i % NP_)
            bst, mv, lnv, rstd, nmr = smalls[i % NP_]
            ks = ("lnsm", i % NP_)
            P.dma(I("dma_start", out=xb, in_=src[tok0 + i * 128: tok0 + (i + 1) * 128, :]), w=[kx])
            for c in range(2):
                bank = (i % 2) * 2 + c
                P.pe(G([MM(PS[bank], BA[:, k, i * 128:(i + 1) * 128], WBIG[:, k, c * 512:(c + 1) * 512], start=(k == 0), stop=(k == 7)) for k in range(8)]),
                     r=["oT", "WBIG"], w=[psk(bank)])
                P.dve(I("scalar_tensor_tensor", out=pre[:, c * 512:(c + 1) * 512], in0=xb[:, c * 512:(c + 1) * 512], scalar=ALPHA, in1=PS[bank], op0=ALU.mult, op1=ALU.add),
                      r=[kx], w=[psk(bank), (kp, c)])
                P.dve(I("bn_stats", out=bst[:, c, :], in_=pre[:, c * 512:(c + 1) * 512]), r=[(kp, c)], w=[(ks, "b", c)])
            P.dve(I("bn_aggr", out=mv, in_=bst), r=[(ks, "b", 0), (ks, "b", 1)], w=[(ks, "mv")])

        def s1(i):
            bst, mv, lnv, rstd, nmr = smalls[i % NP_]
            ks = ("lnsm", i % NP_)
            P.act(I("activation", out=lnv, in_=mv[:, 1:2], func=AF.Ln, bias=epsT[:, 0:1], scale=1.0), r=[(ks, "mv"), "epsT"], w=[(ks, "ln")])
            P.act(I("activation", out=rstd, in_=lnv, func=AF.Exp, scale=-0.5), r=[(ks, "ln")], w=[(ks, "rstd")])

        def s2(i):
            pre, kp = pres[i % NP_], ("pre", i % NP_)
            bst, mv, lnv, rstd, nmr = smalls[i % NP_]
            ks = ("lnsm", i % NP_)
            P.dve(I("tensor_scalar", out=nmr, in0=mv[:, 0:1], scalar1=rstd[:, 0:1], scalar2=-1.0, op0=ALU.mult, op1=ALU.mult), r=[(ks, "mv"), (ks, "rstd")], w=[(ks, "nmr")])
            P.act(I("activation", out=pre, in_=pre, func=AF.Identity, scale=rstd[:, 0:1], bias=nmr[:, 0:1]), r=[(ks, "rstd"), (ks, "nmr")], w=[(kp, 0), (kp, 1)])

        def s3(i):
            pre, kp = pres[i % NP_], ("pre", i % NP_)
            P.pool(I("tensor_tensor", out=pre, in0=pre, in1=gb[:, 0, :], op=ALU.mult), r=["gb"], w=[(kp, 0), (kp, 1)])
            P.pool(I("tensor_tensor", out=pre, in0=pre, in1=gb[:, 1, :], op=ALU.add), r=["gb"], w=[(kp, 0), (kp, 1)])
            P.dma(I("dma_start", out=dst[tok0 + i * 128: tok0 + (i + 1) * 128, :], in_=pre), r=[(kp, 0), (kp, 1)], w=[("dst", i)])

        for n in range(NTL + 3):
            if n < NTL:
                s0(n)
            if 0 <= n - 1 < NTL:
                s1(n - 1)
            if 0 <= n - 2 < NTL:
                s2(n - 2)
            if 0 <= n - 3 < NTL:
                s3(n - 3)

    def mixer_A(b):
        tok0 = b * S
        ar.release(base_mark)
        BK = ar.alloc([8, S], BF16)
        BV = ar.alloc([NTL, D], BF16)
        BKI = ar.alloc([S], BF16)
        BA = ar.alloc([8, S], BF16)
        wiall = ar.alloc([NTL, 8], F32)
        aw = ar.alloc([NTL, 8], F32)
        sg = ar.alloc([NTL, 8], F32)
        m2 = ar.mark()
        WBIG = ar.alloc([8, D], BF16)
        rope = ar.alloc([2, S], F32)
        WC = [ar.alloc([8, 128], BF16) for _ in range(4)]
        Wwi = ar.alloc([8, 8], BF16)
        xs = [ar.alloc([D], F32) for _ in range(2)]
        t1 = [ar.alloc([512], F32) for _ in range(2)]
        t2 = [ar.alloc([512], F32) for _ in range(2)]
        ob = [ar.alloc([512], BF16) for _ in range(2)]

        build_xT(x, tok0, BA, xs)
        P.dma(I("dma_start", out=WBIG, in_=w_in[:, COL_V:COL_V + D].rearrange("(k p) c -> p k c", p=128)), w=["WBIG"], q="pool")
        P.dma(I("dma_start", out=Wwi, in_=w_in[:, COL_WI:COL_WI + 8].rearrange("(k p) c -> p k c", p=128)), w=["Wwi"], q="pool")

        blocks = []
        for j in range(4):
            blocks.append((BLK_QI + j, BLK_QIP + j, 1, ("qi", j)))
        blocks.append((BLK_KI, BLK_KIP, 1, ("ki", 0)))
        for h in range(8):
            blocks.append((BLK_Q + h, BLK_QP + h, 0, ("q", h)))
        for h in range(8):
            blocks.append((BLK_K + h, BLK_KP + h, 0, ("k", h)))
        cur_fam = None
        n = 0
        for bi, (ba_, bp_, fam, dest) in enumerate(blocks):
            if fam != cur_fam:
                cur_fam = fam
                t_c, t_s = (2, 3) if fam == 1 else (0, 1)
                P.dma(I("dma_start", out=rope[:, 0, :], in_=c_rope[t_c]), w=["rope"])
                P.dma(I("dma_start", out=rope[:, 1, :], in_=c_rope[t_s]), w=["rope"])
            wa, wp = WC[2 * (bi % 2)], WC[2 * (bi % 2) + 1]
            ka, kp_ = ("WC", 2 * (bi % 2)), ("WC", 2 * (bi % 2) + 1)
            if bi == 0:
                load_wcol(wa, ka, w_in, ba_ * 128)
                load_wcol(wp, kp_, w_in, bp_ * 128)
            if bi + 1 < len(blocks):
                nb1 = (bi + 1) % 2
                load_wcol(WC[2 * nb1], ("WC", 2 * nb1), w_in, blocks[bi + 1][0] * 128)
                load_wcol(WC[2 * nb1 + 1], ("WC", 2 * nb1 + 1), w_in, blocks[bi + 1][1] * 128)
            for tg in range(NG):
                c0, c1 = tg * 512, (tg + 1) * 512
                pa, pb = (0, 1) if n % 2 == 0 else (2, 3)
                j2 = n % 2
                n += 1
                P.pe(G([MM(PS[pa], wa[:, k, :], BA[:, k, c0:c1], start=(k == 0), stop=(k == 7)) for k in range(8)]), r=[ka] + ba_keys(c0, c1), w=[psk(pa)])
                P.pe(G([MM(PS[pb], wp[:, k, :], BA[:, k, c0:c1], start=(k == 0), stop=(k == 7)) for k in range(8)]), r=[kp_] + ba_keys(c0, c1), w=[psk(pb)])
                P.dve(I("tensor_tensor", out=t1[j2], in0=PS[pa], in1=rope[:, 0, c0:c1], op=ALU.mult), r=["rope"], w=[psk(pa), ("t1", j2)])
                P.dve(I("tensor_tensor", out=t2[j2], in0=PS[pb], in1=rope[:, 1, c0:c1], op=ALU.mult), r=["rope"], w=[psk(pb), ("t2", j2)])
                kind, idx = dest
                if kind == "k":
                    P.pool(I("tensor_tensor", out=BK[:, idx, c0:c1], in0=t1[j2], in1=t2[j2], op=ALU.add), r=[("t1", j2), ("t2", j2)], w=[("BK", idx)])
                elif kind == "ki":
                    P.pool(I("tensor_tensor", out=BKI[:, c0:c1], in0=t1[j2], in1=t2[j2], op=ALU.add), r=[("t1", j2), ("t2", j2)], w=["BKI"])
                else:
                    P.pool(I("tensor_tensor", out=ob[j2], in0=t1[j2], in1=t2[j2], op=ALU.add), r=[("t1", j2), ("t2", j2)], w=[("ob", j2)])
                    dd = qT_d if kind == "q" else qiT_d
                    P.dma(I("dma_start", out=dd[idx, :, c0:c1], in_=ob[j2]), r=[("ob", j2)], w=[(kind + "d", tg)])
        if stop_after == "A.1":
            raise _Stop()
        for i in range(NTL):
            for c in range(2):
                bank = 4 + (2 * i + c) % 4
                P.pe(G([MM(PS[bank], BA[:, k, i * 128:(i + 1) * 128], WBIG[:, k, c * 512:(c + 1) * 512], start=(k == 0), stop=(k == 7)) for k in range(8)]),
                     r=ba_keys(i * 128, (i + 1) * 128) + ["WBIG"], w=[psk(bank)])
                if c == 0:
                    P.act(I("copy", out=BV[:, i, 0:512], in_=PS[bank]), w=[psk(bank), ("BV", i)])
                else:
                    P.dve(I("tensor_copy", out=BV[:, i, 512:1024], in_=PS[bank]), w=[psk(bank), ("BV", i)])
            bank = i % 2
            P.pe(G([MM(PS[bank][:, 0:8], BA[:, k, i * 128:(i + 1) * 128], Wwi[:, k, :], start=(k == 0), stop=(k == 7)) for k in range(8)]),
                 r=ba_keys(i * 128, (i + 1) * 128) + ["Wwi"], w=[psk(bank)])
            P.dve(I("tensor_copy", out=wiall[:, i, :], in_=PS[bank][:, 0:8]), w=[psk(bank), "wiall"])
        P.dve(I("tensor_scalar", out=sg, in0=wiall, scalar1=-IDX_C, scalar2=None, op0=ALU.mult), r=["wiall"], w=["sg"])
        P.dve(I("scalar_tensor_tensor", out=aw, in0=wiall, scalar=IDX_C, in1=sg, op0=ALU.mult, op1=ALU.max), r=["wiall", "sg"], w=["aw"])
        P.dve(I("tensor_scalar", out=sg, in0=wiall, scalar1=0.0, scalar2=2.0, op0=ALU.is_ge, op1=ALU.mult), r=["wiall"], w=["sg"])
        P.dve(I("tensor_scalar", out=sg, in0=sg, scalar1=-1.0, scalar2=None, op0=ALU.add), w=["sg"])

        if stop_after == "A.2":
            raise _Stop()
        P.barrier()
        ar.release(m2)
        qg = [ar.alloc([8, 512], BF16) for _ in range(2)]
        qig = [ar.alloc([4, 512], BF16) for _ in range(2)]
        maskT = [ar.alloc([NTL, 512], BF16) for _ in range(2)]
        score = [ar.alloc([S], F32) for _ in range(2)]
        MB = [ar.alloc([S], BF16) for _ in range(2)]
        Rt = [ar.alloc([512], BF16) for _ in range(4)]
        Dg = [ar.alloc([8, 128], BF16) for _ in range(2)]
        PT = [ar.alloc([512], BF16) for _ in range(3)]
        U = ar.alloc([512], F32)
        Vs = ar.alloc([512], F32)
        bsm = [dict(rmax=ar.alloc([1], F32), rmin=ar.alloc([1], F32), rng=ar.alloc([1], F32), dtab=ar.alloc([NIT], F32),
                    cand=ar.alloc([1], F32), cnt=ar.alloc([1], F32), step=ar.alloc([1], F32), cur=ar.alloc([1], F32)) for _ in range(2)]
        cnts = {"R": 0, "S": 0, "P": 0, "Rt": 0}

        def load_group(g):
            P.dma(I("dma_start", out=qg[g % 2], in_=qT_d[:, :, g * 512:(g + 1) * 512].rearrange("h p s -> p h s")), w=[("qg", g % 2)])
            P.dma(I("dma_start", out=qig[g % 2], in_=qiT_d[:, :, g * 512:(g + 1) * 512].rearrange("h p s -> p h s")), w=[("qig", g % 2)])

        def idx_scores(g, j):
            qigb = qig[g % 2]
            i = 4 * g + j
            L1, L2 = 128 * i + 64, 128 * i + 128
            sc = score[j % 2]
            ksc = ("score", j % 2)
            dg = Dg[j % 2]
            kdg = ("Dg", j % 2)
            P.dve(I("tensor_tensor", out=dg, in0=identB.unsqueeze(1).to_broadcast([128, 8, 128]), in1=sg[:, i, :].unsqueeze(2).to_broadcast([128, 8, 128]), op=ALU.mult),
                  r=["identB", "sg"], w=[kdg])
            steps = []
            for kr in range(0, L2, 512):
                for h in range(8):
                    steps.append((kr, min(512, L2 - kr), h))
            info = {}

            def rel(n):
                kr, w_, h = steps[n]
                hp = (h % 2) * 64
                bank = cnts["R"] % 2
                cnts["R"] += 1
                info[n] = bank
                P.pe(I("matmul", out=PS[bank][:, 0:w_], lhsT=qigb[hp:hp + 64, h // 2, j * 128:(j + 1) * 128], rhs=BKI[hp:hp + 64, kr:kr + w_], start=True, stop=True),
                     r=[("qig", g % 2), "BKI"], w=[psk(bank)])

            def relu_acc(n):
                kr, w_, h = steps[n]
                bank = info[n]
                rt = Rt[cnts["Rt"] % 4]
                krt = ("Rt", cnts["Rt"] % 4)
                cnts["Rt"] += 1
                P.act(I("activation", out=rt[:, 0:w_], in_=PS[bank][:, 0:w_], func=AF.Relu, scale=aw[:, i, h:h + 1]), r=["aw"], w=[psk(bank), krt])
                P.pe(I("matmul", out=PS[7][:, 0:w_], lhsT=dg[:, h, :], rhs=rt[:, 0:w_], start=(h == 0), stop=(h == 7)), r=[krt, kdg], w=[psk(7)])
                if h == 7:
                    P.dve(I("tensor_copy", out=sc[:, kr:kr + w_], in_=PS[7][:, 0:w_]), w=[psk(7), ksc])
            rel(0)
            for n in range(len(steps)):
                if n + 1 < len(steps):
                    rel(n + 1)
                relu_acc(n)
            P.dve(I("memset", sc[0:64, L1:L2], -3.0e38), w=[ksc])

        def bis_ops(g, j):
            i = 4 * g + j
            L1, L2 = 128 * i + 64, 128 * i + 128
            sc, mb, sm = score[j % 2], MB[j % 2], bsm[j % 2]
            ksc, kb_, kmb, kcur = ("score", j % 2), ("bis", j % 2), ("MB", j % 2), ("cur", j % 2)
            ops = []
            if L1 <= TOPK:
                ops.append(lambda: P.dve(I("memset", sm["cur"], -1.0e30), w=[kcur]))
                return ops
            ops.append(lambda: P.dve(I("tensor_reduce", out=sm["rmax"], in_=sc[:, 0:L2], axis=AX.X, op=ALU.max), r=[ksc], w=[kb_]))
            ops.append(lambda: P.dve(I("tensor_reduce", out=sm["rmin"], in_=sc[:, 0:L1], axis=AX.X, op=ALU.min), r=[ksc], w=[kb_]))
            ops.append(lambda: P.dve(I("tensor_tensor", out=sm["rng"], in0=sm["rmax"], in1=sm["rmin"], op=ALU.subtract), w=[kb_]))
            ops.append(lambda: P.dve(I("tensor_scalar", out=sm["dtab"], in0=pw, scalar1=sm["rng"][:, 0:1], scalar2=None, op0=ALU.mult), r=["pw"], w=[kb_]))
            ops.append(lambda: P.dve(I("tensor_tensor", out=sm["cand"], in0=sm["rmin"], in1=sm["dtab"][:, 0:1], op=ALU.add), w=[kb_]))
            for it in range(NIT):
                ops.append(lambda: P.dve(I("tensor_scalar", out=mb[:, 0:L2], in0=sc[:, 0:L2], scalar1=sm["cand"][:, 0:1], scalar2=0.0, op0=ALU.is_ge, op1=ALU.add, accum_out=sm["cnt"]),
                                         r=[ksc], w=[kb_, kmb]))
                ops.append(lambda it=it: P.dve(I("tensor_scalar", out=sm["step"], in0=sm["cnt"], scalar1=float(TOPK) - 0.5, scalar2=sm["dtab"][:, it:it + 1], op0=ALU.is_ge, op1=ALU.mult), w=[kb_]))
                if it < NIT - 1:
                    ops.append(lambda it=it: P.dve(I("scalar_tensor_tensor", out=sm["cand"], in0=sm["step"], scalar=sm["dtab"][:, it + 1:it + 2], in1=sm["cand"], op0=ALU.subtract, op1=ALU.add), w=[kb_]))
                else:
                    ops.append(lambda it=it: P.dve(I("scalar_tensor_tensor", out=sm["cur"], in0=sm["step"], scalar=sm["dtab"][:, it:it + 1], in1=sm["cand"], op0=ALU.subtract, op1=ALU.add), w=[kb_, kcur]))
            return ops

        def mask_tile(g, j):
            i = 4 * g + j
            L2 = 128 * i + 128
            sc, mb, sm = score[j % 2], MB[j % 2], bsm[j % 2]
            mT = maskT[g % 2]
            P.dve(I("tensor_scalar", out=mb[:, 0:L2], in0=sc[:, 0:L2], scalar1=sm["cur"][:, 0:1], scalar2=NEG, op0=ALU.is_lt, op1=ALU.mult),
                  r=[("score", j % 2), ("cur", j % 2)], w=[("MB", j % 2)])
            for kb0 in range(0, i + 1, 4):
                nb = min(4, i + 1 - kb0)
                bank = cnts["R"] % 2
                cnts["R"] += 1
                P.pe(G([("transpose", dict(out=PSB[bank][:, q * 128:(q + 1) * 128], in_=mb[:, (kb0 + q) * 128:(kb0 + q + 1) * 128], identity=identB)) for q in range(nb)]),
                     r=[("MB", j % 2), "identB"], w=[psk(bank)])
                P.act(I("copy", out=mT[:, kb0:kb0 + nb, j * 128:(j + 1) * 128], in_=PSB[bank][:, 0:nb * 128].rearrange("p (a b) -> p a b", a=nb)),
                      w=[psk(bank), ("maskT", g % 2, j)])

        def att_heads(g, heads):
            qgb = qg[g % 2]
            mT = maskT[g % 2]
            nkb = 4 * g + 4
            bo, bs = 5, 6
            blks = [(h, kb) for h in heads for kb in range(nkb)]
            info = {}

            def sS(n):
                h, kb = blks[n]
                jl = max(0, kb - 4 * g)
                q0 = jl * 128
                N = 512 - q0
                sb_ = 2 + (cnts["S"] % 3)
                cnts["S"] += 1
                info[n] = sb_
                P.pe(G([MM(PS[sb_][:, 0:N], BK[:, h, kb * 128:(kb + 1) * 128], qgb[:, h, q0:512], start=True, stop=False),
                        MM(PS[sb_][:, 0:N], identB, mT[:, kb, q0:512], start=False, stop=True)]),
                     r=[("BK", h), ("qg", g % 2), "identB"] + [("maskT", g % 2, jj) for jj in range(jl, 4)], w=[psk(sb_)])

            def sPV(n):
                h, kb = blks[n]
                jl = max(0, kb - 4 * g)
                q0 = jl * 128
                N = 512 - q0
                sb_ = info[n]
                pt = PT[cnts["P"] % 3]
                kpt = ("PT", cnts["P"] % 3)
                cnts["P"] += 1
                P.act(I("activation", out=pt[:, 0:N], in_=PS[sb_][:, 0:N], func=AF.Exp, scale=QK_SCALE), w=[psk(sb_), kpt])
                P.pe(G([MM(PS[bo][:, q0:512], BV[:, kb, h * 128:(h + 1) * 128], pt[:, 0:N], start=(kb == 0), stop=(kb == nkb - 1)),
                        MM(PS[bs][:, q0:512], onesB, pt[:, 0:N], start=(kb == 0), stop=(kb == nkb - 1))]),
                     r=[kpt, ("BV", kb), "onesB"], w=[psk(bo), psk(bs)])
                if kb == nkb - 1:
                    P.act(I("copy", out=U, in_=PS[bo]), w=[psk(bo), "U"])
                    P.act(I("activation", out=Vs, in_=PS[bs], func=AF.Ln), w=[psk(bs), "Vs"])
                    P.act(I("activation", out=Vs, in_=Vs, func=AF.Exp, scale=-1.0), w=["Vs"])
                    P.pool(I("tensor_tensor", out=BA[:, h, g * 512:(g + 1) * 512], in0=U, in1=Vs, op=ALU.mult), r=["U", "Vs"], w=["oT"])
            sS(0)
            for n in range(len(blks)):
                if n + 1 < len(blks):
                    sS(n + 1)
                sPV(n)

        for g in range(NG + 1):
            if g < NG:
                load_group(g)
            for pr in range(2):
                if g < NG:
                    idx_scores(g, 2 * pr)
                    idx_scores(g, 2 * pr + 1)
                    la, lb = bis_ops(g, 2 * pr), bis_ops(g, 2 * pr + 1)
                    for k in range(max(len(la), len(lb))):
                        if k < len(la):
                            la[k]()
                        if k < len(lb):
                            lb[k]()
                if g >= 1:
                    att_heads(g - 1, list(range(4 * pr, 4 * pr + 4)))
                if g < NG:
                    mask_tile(g, 2 * pr)
                    mask_tile(g, 2 * pr + 1)
        if stop_after == "A.3":
            raise _Stop()
        P.barrier()
        ar.release(m2)
        WBIG = ar.alloc([8, D], BF16)
        P.dma(I("dma_start", out=WBIG, in_=a_w_out.rearrange("(k p) c -> p k c", p=128)), w=["WBIG"], q="pool")
        gb = ar.alloc([2, D], F32)
        load_gb(gb, 0)
        xs2 = [ar.alloc([D], F32) for _ in range(3)]
        pres = [ar.alloc([D], F32) for _ in range(5)]
        smalls = [ln_smalls() for _ in range(5)]
        out_proj_ln(BA, WBIG, x, h1, tok0, 0, xs2, pres, gb, smalls)
        P.barrier()

    def mixer_B(b):
        tok0 = b * S
        ar.release(base_mark)
        BK = ar.alloc([8, S], BF16)
        BQ = ar.alloc([8, S], BF16)
        BV = ar.alloc([NTL, D], BF16)
        BA = ar.alloc([8, S], BF16)
        WBIG = ar.alloc([8, D], BF16)
        m2 = ar.mark()
        WC = [ar.alloc([8, 128], BF16) for _ in range(2)]
        xs = [ar.alloc([D], F32) for _ in range(3)]
        build_xT(h2, tok0, BA, xs)
        P.dma(I("dma_start", out=WBIG, in_=b_w_kv[:, D:2 * D].rearrange("(k p) c -> p k c", p=128)), w=["WBIG"], q="pool")
        n = 0
        for bi in range(16):
            wap, c0w, dstB, kd = (b_w_q, bi * 128, BQ, ("BQ", bi)) if bi < 8 else (b_w_kv, (bi - 8) * 128, BK, ("BK", bi - 8))
            wa, ka = WC[bi % 2], ("WC", bi % 2)
            if bi == 0:
                load_wcol(wa, ka, wap, c0w)
            if bi + 1 < 16:
                bn = bi + 1
                wapn, c0n = (b_w_q, bn * 128) if bn < 8 else (b_w_kv, (bn - 8) * 128)
                load_wcol(WC[bn % 2], ("WC", bn % 2), wapn, c0n)
            for tg in range(NG):
                c0, c1 = tg * 512, (tg + 1) * 512
                pa = n % 4
                n += 1
                P.pe(G([MM(PS[pa], wa[:, k, :], BA[:, k, c0:c1], start=(k == 0), stop=(k == 7)) for k in range(8)]), r=[ka] + ba_keys(c0, c1), w=[psk(pa)])
                if n % 2 == 0:
                    P.act(I("copy", out=dstB[:, bi % 8, c0:c1], in_=PS[pa]), w=[psk(pa), kd])
                else:
                    P.dve(I("tensor_copy", out=dstB[:, bi % 8, c0:c1], in_=PS[pa]), w=[psk(pa), kd])
        for i in range(NTL):
            for c in range(2):
                bank = 4 + (2 * i + c) % 4
                P.pe(G([MM(PS[bank], BA[:, k, i * 128:(i + 1) * 128], WBIG[:, k, c * 512:(c + 1) * 512], start=(k == 0), stop=(k == 7)) for k in range(8)]),
                     r=ba_keys(i * 128, (i + 1) * 128) + ["WBIG"], w=[psk(bank)])
                if c == 0:
                    P.act(I("copy", out=BV[:, i, 0:512], in_=PS[bank]), w=[psk(bank), ("BV", i)])
                else:
                    P.dve(I("tensor_copy", out=BV[:, i, 512:1024], in_=PS[bank]), w=[psk(bank), ("BV", i)])
        P.barrier()
        ar.release(m2)
        NEB = 6
        E = [ar.alloc([512], F32) for _ in range(NEB)]
        SPb = [ar.alloc([512], BF16) for _ in range(NEB)]
        X = [ar.alloc([512], F32) for _ in range(3)]
        A = [ar.alloc([512], BF16) for _ in range(3)]
        m3 = ar.mark()
        blks = []
        for g in range(NG):
            kbs = list(range(4 * g + 3, -1, -1))
            for hp_ in range(0, 8, 2):
                for ii, kb in enumerate(kbs):
                    for c in range(2):
                        blks.append(dict(h=hp_ + c, g=g, kb=kb, first=(ii == 0), last=(ii == len(kbs) - 1), grp=c))
        nb_ = len(blks)

        def geo(bk):
            jl = max(0, bk["kb"] - 4 * bk["g"])
            return jl * 128, 512 - jl * 128, bk["kb"] >= 4 * bk["g"]

        def sZ(n):
            bk = blks[n]
            q0, N, diag = geo(bk)
            h, g, kb = bk["h"], bk["g"], bk["kb"]
            bz = n % 3
            P.pe(I("matmul", out=PS[bz][:, 0:N], lhsT=BK[:, h, kb * 128:(kb + 1) * 128], rhs=BQ[:, h, g * 512 + q0:(g + 1) * 512], start=True, stop=True),
                 r=[("BK", h), ("BQ", h)], w=[psk(bz)])

        def sE(n):
            bk = blks[n]
            q0, N, diag = geo(bk)
            bz = n % 3
            e_, sp_ = E[n % NEB], SPb[n % NEB]
            ke, ksp = ("E", n % NEB), ("SP", n % NEB)
            P.act(I("activation", out=e_[:, 0:N], in_=PS[bz][:, 0:N], func=AF.Exp, scale=QK_SCALE), w=[psk(bz), ke])
            P.act(I("activation", out=sp_[:, 0:N], in_=e_[:, 0:N], func=AF.Ln, bias=onesF[:, 0:1], scale=1.0), r=[ke, "onesF"], w=[ksp])
            if diag:
                P.pool(I("tensor_tensor", out=sp_[:, 0:128], in0=sp_[:, 0:128], in1=causF, op=ALU.mult), r=["causF"], w=[ksp])

        def sRa(n):
            bk = blks[n]
            q0, N, diag = geo(bk)
            br = 3 + bk["grp"]
            P.pe(I("matmul", out=PS[br][:, q0:512], lhsT=triB, rhs=SPb[n % NEB][:, 0:N], start=bk["first"], stop=False, skip_group_check=True),
                 r=[("SP", n % NEB), "triB"], w=[psk(br)])

        def sX(n):
            bk = blks[n]
            q0, N, diag = geo(bk)
            br = 3 + bk["grp"]
            P.act(I("activation", out=X[n % 3][:, 0:N], in_=PS[br][:, q0:512], func=AF.Exp, scale=-1.0), w=[psk(br), ("X", n % 3)])

        def sRbA(n):
            bk = blks[n]
            q0, N, diag = geo(bk)
            br = 3 + bk["grp"]
            if not bk["last"]:
                P.pe(I("matmul", out=PS[br][:, q0:512], lhsT=u2B, rhs=SPb[n % NEB][:, 0:N], start=False, stop=False, skip_group_check=True),
                     r=[("SP", n % NEB), "u2B"], w=[psk(br)])
            a_ = A[n % 3]
            P.dve(I("tensor_tensor", out=a_[:, 0:N], in0=E[n % NEB][:, 0:N], in1=X[n % 3][:, 0:N], op=ALU.mult), r=[("E", n % NEB), ("X", n % 3)], w=[("A", n % 3)])
            if diag:
                P.pool(I("tensor_tensor", out=a_[:, 0:128], in0=a_[:, 0:128], in1=causF, op=ALU.mult), r=["causF"], w=[("A", n % 3)])

        def sO(n):
            bk = blks[n]
            q0, N, diag = geo(bk)
            h, g, kb = bk["h"], bk["g"], bk["kb"]
            bo = 5 + bk["grp"]
            P.pe(I("matmul", out=PS[bo][:, q0:512], lhsT=BV[:, kb, h * 128:(h + 1) * 128], rhs=A[n % 3][:, 0:N], start=bk["first"], stop=bk["last"], skip_group_check=True),
                 r=[("A", n % 3), ("BV", kb)], w=[psk(bo)])
            if bk["last"]:
                if bk["grp"] == 0:
                    P.act(I("copy", out=BA[:, h, g * 512:(g + 1) * 512], in_=PS[bo]), w=[psk(bo), "oT"])
                else:
                    P.dve(I("tensor_copy", out=BA[:, h, g * 512:(g + 1) * 512], in_=PS[bo]), w=[psk(bo), "oT"])

        for n in range(nb_ + 5):
            if n < nb_:
                sZ(n)
            if 0 <= n - 4 < nb_:
                sRbA(n - 4)
            if 0 <= n - 1 < nb_:
                sE(n - 1)
            if 0 <= n - 2 < nb_:
                sRa(n - 2)
            if 0 <= n - 3 < nb_:
                sX(n - 3)
            if 0 <= n - 5 < nb_:
                sO(n - 5)
        P.barrier()
        ar.release(m2)
        P.dma(I("dma_start", out=WBIG, in_=b_w_out.rearrange("(k p) c -> p k c", p=128)), w=["WBIG"], q="pool")
        gb = ar.alloc([2, D], F32)
        load_gb(gb, 2)
        xs2 = [ar.alloc([D], F32) for _ in range(3)]
        pres = [ar.alloc([D], F32) for _ in range(5)]
        smalls = [ln_smalls() for _ in range(5)]
        out_proj_ln(BA, WBIG, h2, h3, tok0, 2, xs2, pres, gb, smalls)
        P.barrier()

    def moe(layer, src, dst, lnidx):
        SG = S
        NTS = SG // 128
        for sgi in range(NT // SG):
            tok0 = sgi * SG
            ar.release(base_mark)
            hTb = ar.alloc([8, SG], BF16)
            ysb = ar.alloc([NTS, D], F32)
            gates = ar.alloc([NTS, NE], F32)
            Wg = [ar.alloc([8, DFF], BF16) for _ in range(2)]
            Wu = [ar.alloc([8, DFF], BF16) for _ in range(2)]
            Wd = [ar.alloc([2, D], BF16) for _ in range(2)]
            gb = ar.alloc([2, D], F32)
            load_gb(gb, lnidx)
            m2 = ar.mark()
            hs = [ar.alloc([D], F32) for _ in range(3)]
            hT32 = [ar.alloc([8, 128], F32) for _ in range(3)]
            sgt = [ar.alloc([512], F32) for _ in range(2)]
            he = [ar.alloc([2, 512], BF16) for _ in range(3)]
            stg = [ar.alloc([8, DFF], F32), ar.alloc([8, DFF], F32), ar.alloc([2, D], F32)]
            lg_all = ar.alloc([NTS, NE], F32)
            ex = ar.alloc([NTS, NE], F32)
            pr = ar.alloc([NTS, NE], F32)
            sel = ar.alloc([NTS, NE], F32)
            gt = ar.alloc([NTS, NE], F32)
            mx = ar.alloc([NTS], F32)
            se = ar.alloc([NTS], F32)
            q4 = [ar.alloc([NTS, 4], F32) for _ in range(8)]
            bst_all = ar.alloc([NTS, 2, 6], F32)
            mv_all = ar.alloc([NTS, 2], F32)
            rstd_all = ar.alloc([NTS], F32)
            nmr_all = ar.alloc([NTS], F32)
            def r0(i):
                hb, kh = hs[i % 3], ("hs", i % 3)
                h32, k32 = hT32[i % 3], ("hT32", i % 3)
                P.dma(I("dma_start", out=hb, in_=src[tok0 + i * 128: tok0 + (i + 1) * 128, :]), w=[kh])
                ba, bb = (0, 1) if i % 2 == 0 else (2, 3)
                calls = []
                for k in range(8):
                    bank = ba if k < 4 else bb
                    calls.append(("transpose", dict(out=PS[bank][:, (k % 4) * 128:(k % 4 + 1) * 128], in_=hb[:, k * 128:(k + 1) * 128], identity=identF)))
                P.pe(G(calls), r=[kh, "identF"], w=[psk(ba), psk(bb)])
                P.act(I("copy", out=h32[:, 0:4, :], in_=PS[ba].rearrange("p (a b) -> p a b", a=4)), w=[psk(ba), (k32, 0)])
                P.dve(I("tensor_copy", out=h32[:, 4:8, :], in_=PS[bb].rearrange("p (a b) -> p a b", a=4)), w=[psk(bb), (k32, 1)])

            def r1(i):
                hb, kh = hs[i % 3], ("hs", i % 3)
                h32, k32 = hT32[i % 3], ("hT32", i % 3)
                P.pool(I("tensor_copy", out=hTb[:, :, i * 128:(i + 1) * 128], in_=h32), r=[(k32, 0), (k32, 1)], w=[("hTb", i)])
                P.pool(I("tensor_scalar", out=ysb[:, i, :], in0=hb, scalar1=ALPHA, scalar2=None, op0=ALU.mult), r=[kh], w=[("ysb", i)])
                bk = 4 + i % 2
                P.pe(G([MM(PS[bk][:, 0:NE], h32[:, k, :], rw32[:, k, :], start=(k == 0), stop=(k == 7)) for k in range(8)]), r=[(k32, 0), (k32, 1), "rw32"], w=[psk(bk)])
                P.dve(I("tensor_copy", out=lg_all[:, i, :], in_=PS[bk][:, 0:NE]), w=[psk(bk), ("lg", i)])
            for n in range(NTS + 1):
                if n < NTS:
                    r0(n)
                if n >= 1:
                    r1(n - 1)
            kg = "gat"
            lgk = [("lg", i) for i in range(NTS)]
            B3 = [128, NTS, NE]
            B4 = [128, NTS, 4, 4]

            def v4(t):
                return t.rearrange("p n (a b) -> p n a b", a=4)
            P.dve(I("tensor_reduce", out=mx, in_=lg_all, axis=AX.X, op=ALU.max), r=lgk, w=[kg])
            P.dve(I("tensor_tensor", out=ex, in0=lg_all, in1=mx.unsqueeze(2).to_broadcast(B3), op=ALU.subtract), r=lgk, w=[kg])
            P.act(I("activation", out=ex, in_=ex, func=AF.Exp), w=[kg])
            P.dve(I("tensor_reduce", out=se, in_=ex, axis=AX.X, op=ALU.add), w=[kg])
            P.dve(I("reciprocal", out=se, in_=se), w=[kg])
            P.dve(I("tensor_tensor", out=pr, in0=ex, in1=se.unsqueeze(2).to_broadcast(B3), op=ALU.mult), w=[kg])
            P.dve(I("tensor_tensor", out=sel, in0=pr, in1=rbias.unsqueeze(1).to_broadcast(B3), op=ALU.add), r=["rbias"], w=[kg])
            s4 = v4(sel)
            a_, b_, c_, d_ = s4[:, :, :, 0], s4[:, :, :, 1], s4[:, :, :, 2], s4[:, :, :, 3]
            P.dve(I("tensor_tensor", out=q4[0], in0=a_, in1=b_, op=ALU.max), w=[kg])
            P.dve(I("tensor_tensor", out=q4[1], in0=a_, in1=b_, op=ALU.min), w=[kg])
            P.dve(I("tensor_tensor", out=q4[2], in0=c_, in1=d_, op=ALU.max), w=[kg])
            P.dve(I("tensor_tensor", out=q4[3], in0=c_, in1=d_, op=ALU.min), w=[kg])
            P.dve(I("tensor_tensor", out=q4[4], in0=q4[0], in1=q4[2], op=ALU.max), w=[kg])
            P.dve(I("tensor_tensor", out=q4[5], in0=q4[0], in1=q4[2], op=ALU.min), w=[kg])
            P.dve(I("tensor_tensor", out=q4[6], in0=q4[1], in1=q4[3], op=ALU.max), w=[kg])
            P.dve(I("tensor_tensor", out=q4[7], in0=q4[5], in1=q4[6], op=ALU.max), w=[kg])
            P.dve(I("tensor_tensor", out=q4[0], in0=q4[4], in1=q4[7], op=ALU.add), w=[kg])
            P.dve(I("tensor_reduce", out=mx, in_=q4[0], axis=AX.X, op=ALU.max), w=[kg])
            P.dve(I("tensor_tensor", out=q4[1], in0=q4[0], in1=mx.unsqueeze(2).to_broadcast([128, NTS, 4]), op=ALU.is_ge), w=[kg])
            P.dve(I("tensor_tensor", out=v4(gt), in0=s4, in1=q4[7].unsqueeze(3).to_broadcast(B4), op=ALU.is_ge), w=[kg])
            P.dve(I("tensor_tensor", out=v4(gt), in0=v4(gt), in1=q4[1].unsqueeze(3).to_broadcast(B4), op=ALU.mult), w=[kg])
            P.dve(I("tensor_tensor", out=gt, in0=gt, in1=pr, op=ALU.mult), w=[kg])
            P.dve(I("tensor_reduce", out=se, in_=gt, axis=AX.X, op=ALU.add), w=[kg])
            P.dve(I("reciprocal", out=se, in_=se), w=[kg])
            P.dve(I("tensor_tensor", out=gates, in0=gt, in1=se.unsqueeze(2).to_broadcast(B3), op=ALU.mult), w=[kg, "gates"])
            steps = [(e, tg) for e in range(NE) for tg in range(SG // 512)]
            nY = [0]

            def load_w(e):
                kw = ("Wexp", e % 2)
                P.dma(I("dma_start", out=stg[0], in_=w_gate[layer, e].rearrange("(k p) f -> p k f", p=128)), w=[("stg", 0)])
                P.pool(I("tensor_copy", out=Wg[e % 2], in_=stg[0]), r=[("stg", 0)], w=[kw])
                P.dma(I("dma_start", out=stg[1], in_=w_up[layer, e].rearrange("(k p) f -> p k f", p=128)), w=[("stg", 1)])
                P.pool(I("tensor_copy", out=Wu[e % 2], in_=stg[1]), r=[("stg", 1)], w=[kw])
                P.dma(I("dma_start", out=stg[2], in_=w_down[layer, e].rearrange("(c p) d -> p c d", p=128)), w=[("stg", 2)])
                P.pool(I("tensor_copy", out=Wd[e % 2], in_=stg[2]), r=[("stg", 2)], w=[kw])

            def gu(n):
                e, tg = steps[n]
                wg, wu = Wg[e % 2], Wu[e % 2]
                kw = ("Wexp", e % 2)
                c0, c1 = tg * 512, (tg + 1) * 512
                heb = he[n % 3]
                khe = ("he", n % 3)
                hk = [("hTb", t) for t in range(tg * 4, tg * 4 + 4)]
                for fc in range(2):
                    pg, pu = 2 * fc, 2 * fc + 1
                    P.pe(G([MM(PS[pg], wg[:, k, fc * 128:(fc + 1) * 128], hTb[:, k, c0:c1], start=(k == 0), stop=(k == 7)) for k in range(8)] +
                           [MM(PS[pu], wu[:, k, fc * 128:(fc + 1) * 128], hTb[:, k, c0:c1], start=(k == 0), stop=(k == 7)) for k in range(8)]),
                         r=[kw] + hk, w=[psk(pg), psk(pu)])

            def gu_post(n, fcs=(0, 1)):
                heb = he[n % 3]
                khe = ("he", n % 3)
                for fc in fcs:
                    pg, pu = 2 * fc, 2 * fc + 1
                    st_ = sgt[fc]
                    kst = ("sgt", fc)
                    P.act(I("activation", out=st_, in_=PS[pg], func=AF.Silu), w=[psk(pg), kst])
                    P.dve(I("tensor_tensor", out=heb[:, fc, :], in0=st_, in1=PS[pu], op=ALU.mult), r=[kst], w=[psk(pu), khe])

            def down(n, jts=(0, 1, 2, 3)):
                e, tg = steps[n]
                wd = Wd[e % 2]
                kw = ("Wexp", e % 2)
                heb = he[n % 3]
                khe = ("he", n % 3)
                for jt in jts:
                    i = tg * 4 + jt
                    for c in range(2):
                        by = 4 + nY[0] % 4
                        nY[0] += 1
                        P.pe(G([MM(PS[by], heb[:, fc, jt * 128:(jt + 1) * 128], wd[:, fc, c * 512:(c + 1) * 512], start=(fc == 0), stop=(fc == 1)) for fc in range(2)]),
                             r=[khe, kw], w=[psk(by)])
                        P.dve(I("scalar_tensor_tensor", out=ysb[:, i, c * 512:(c + 1) * 512], in0=PS[by], scalar=gates[:, i, e:e + 1], in1=ysb[:, i, c * 512:(c + 1) * 512], op0=ALU.mult, op1=ALU.add),
                              r=["gates"], w=[psk(by), ("ysb", i)])
                if tg == SG // 512 - 1 and e + 2 < NE and 3 in jts:
                    load_w(e + 2)
            load_w(0)
            load_w(1)
            gu(0)
            gu_post(0)
            for n in range(len(steps)):
                if n + 1 < len(steps):
                    gu(n + 1)
                down(n, (0, 1))
                if n + 1 < len(steps):
                    gu_post(n + 1, (0,))
                down(n, (2, 3))
                if n + 1 < len(steps):
                    gu_post(n + 1, (1,))
            for i in range(NTS):
                P.dve(I("bn_stats", out=bst_all[:, i, 0, :], in_=ysb[:, i, 0:512]), r=[("ysb", i)], w=[("bst", i, 0)])
                P.dve(I("bn_stats", out=bst_all[:, i, 1, :], in_=ysb[:, i, 512:1024]), r=[("ysb", i)], w=[("bst", i, 1)])
                P.dve(I("bn_aggr", out=mv_all[:, i, :], in_=bst_all[:, i]), r=[("bst", i, 0), ("bst", i, 1)], w=[("mv", i)])
            mvk = [("mv", i) for i in range(NTS)]
            P.act(I("activation", out=rstd_all, in_=mv_all[:, :, 1], func=AF.Ln, bias=epsT[:, 0:1], scale=1.0), r=mvk + ["epsT"], w=["rstd"])
            P.act(I("activation", out=rstd_all, in_=rstd_all, func=AF.Exp, scale=-0.5), w=["rstd"])
            P.dve(I("scalar_tensor_tensor", out=nmr_all, in0=mv_all[:, :, 0], scalar=-1.0, in1=rstd_all, op0=ALU.mult, op1=ALU.mult), r=mvk + ["rstd"], w=["nmr"])
            for i in range(NTS):
                kp = ("ysb", i)
                yi = ysb[:, i, :]
                P.act(I("activation", out=yi, in_=yi, func=AF.Identity, scale=rstd_all[:, i:i + 1], bias=nmr_all[:, i:i + 1]), r=["rstd", "nmr"], w=[kp])
                P.pool(I("tensor_tensor", out=yi, in0=yi, in1=gb[:, 0, :], op=ALU.mult), r=["gb"], w=[kp])
                P.pool(I("tensor_tensor", out=yi, in0=yi, in1=gb[:, 1, :], op=ALU.add), r=["gb"], w=[kp])
                P.dma(I("dma_start", out=dst[tok0 + i * 128: tok0 + (i + 1) * 128, :], in_=yi), r=[kp], w=[("dst", i)])
            P.barrier()

    stages = [("A", lambda: [mixer_A(b) for b in range(NSEQ)]),
              ("M0", lambda: moe(0, h1, h2, 1)),
              ("B", lambda: [mixer_B(b) for b in range(NSEQ)]),
              ("M1", lambda: moe(1, h3, out, 3))]
    try:
        for name, fn in stages:
            fn()
            if stop_after == name:
                break
    except _Stop:
        pass
    P.emit(nc)
    es.close()
    nc._prog_stats = {e: len(P.eng_ops[e]) for e in ENGS}
    return nc


def rope_tables(S):
    def tab(dim, rep):
        inv = (1.0 / (10000.0 ** (np.arange(0, dim, 2, dtype=np.float32) / np.float32(dim)))).astype(np.float32)
        ang = (np.arange(S, dtype=np.float32)[:, None] * inv[None, :]).astype(np.float32)
        c = np.cos(ang).astype(np.float32).T
        s = np.sin(ang).astype(np.float32).T
        cosT = np.concatenate([c, c], 0)
        sinT = np.concatenate([-s, s], 0)
        return np.tile(cosT, (rep, 1)), np.tile(sinT, (rep, 1))
    c128, s128 = tab(128, 1)
    c64, s64 = tab(64, 2)
    return np.ascontiguousarray(np.stack([c128, s128, c64, s64], 0).astype(np.float32))


def host_consts(S):
    ii = np.arange(128)
    return {
        "c_ident": np.eye(128, dtype=np.float32),
        "c_tri": (ii[:, None] >= ii[None, :]).astype(np.float32),
        "c_caus": (ii[:, None] < ii[None, :]).astype(np.float32),
        "c_rope": rope_tables(S),
    }


def make_in_maps(inputs, S, NSEQ, ncores):
    x = np.asarray(inputs["x"], dtype=np.float32)
    cols = a_ext_cols()
    shared = {
        "w_in_ext": np.ascontiguousarray(np.asarray(inputs["a_w_in"], np.float32)[0][:, cols]),
        "a_w_out": np.ascontiguousarray(np.asarray(inputs["a_w_out"], np.float32)[0]),
        "b_w_q": np.ascontiguousarray(np.asarray(inputs["b_w_q"], np.float32)[0]),
        "b_w_kv": np.ascontiguousarray(np.asarray(inputs["b_w_kv"], np.float32)),
        "b_w_out": np.ascontiguousarray(np.asarray(inputs["b_w_out"], np.float32)[0]),
        "router_w": np.ascontiguousarray(np.asarray(inputs["router_w"], np.float32)),
        "router_bias": np.ascontiguousarray(np.asarray(inputs["router_bias"], np.float32).reshape(1, NE)),
        "exp_w_gate": np.ascontiguousarray(np.asarray(inputs["exp_w_gate"], np.float32)),
        "exp_w_up": np.ascontiguousarray(np.asarray(inputs["exp_w_up"], np.float32)),
        "exp_w_down": np.ascontiguousarray(np.asarray(inputs["exp_w_down"], np.float32)),
        "ln_g": np.ascontiguousarray(np.asarray(inputs["ln_g"], np.float32).reshape(4, D)),
        "ln_b": np.ascontiguousarray(np.asarray(inputs["ln_b"], np.float32).reshape(4, D)),
    }
    shared.update(host_consts(S))
    maps = []
    for c in range(ncores):
        m = dict(shared)
        m["x"] = np.ascontiguousarray(x[c * NSEQ:(c + 1) * NSEQ].reshape(NSEQ * S, D))
        maps.append(m)
    return maps


_NC_CACHE = {}


def kernel(**inputs):
    x = np.asarray(inputs["x"])
    B, S, _ = x.shape
    ncores = 8
    NSEQ = B // ncores
    key = (S, NSEQ)
    if key not in _NC_CACHE:
        _NC_CACHE[key] = build(S, NSEQ, min(256, S // 4))
    nc = _NC_CACHE[key]
    maps = make_in_maps(inputs, S, NSEQ, ncores)
    res = run_bass_kernel_spmd(nc, maps, core_ids=list(range(ncores)))
    outs = [np.asarray(r["out"], dtype=np.float32).reshape(NSEQ, S, D) for r in res.results]
    return np.concatenate(outs, axis=0)
```

```python
import math
from contextlib import ExitStack

import numpy as np

import concourse.bass as bass
import concourse.mybir as mybir
from concourse.bass_utils import run_bass_kernel_spmd

F32 = mybir.dt.float32
BF16 = mybir.dt.bfloat16
AF = mybir.ActivationFunctionType
ALU = mybir.AluOpType
AX = mybir.AxisListType

D = 1024
NH = 8
HD = 128
NE = 16
DFF = 256
ALPHA = 4.0 ** 0.25
LN_EPS = 1e-5
NEG = -30000.0
QK_SCALE = HD ** -0.5
IDX_C = (8 ** -0.5) * (64 ** -0.5)
NIT = 18

ENGS = ("pe", "act", "dve", "pool", "sp")
NRING = 12


class _Op:
    __slots__ = ("id", "eng", "fn", "deps", "is_dma", "ring", "ringval", "marked", "val", "pos")

    def __init__(self, id, eng, fn, is_dma):
        self.id = id
        self.eng = eng
        self.fn = fn
        self.deps = set()
        self.is_dma = is_dma
        self.ring = None
        self.ringval = 0
        self.marked = False
        self.val = 0
        self.pos = 0


class Prog:
    def __init__(self):
        self.ops = []
        self.lastw = {}
        self.readers = {}
        self.eng_ops = {e: [] for e in ENGS}
        self.ring_cnt = {}
        self.ring_last = {}
        self.dma_n = {e: 0 for e in ENGS}
        self._bar_pending = {}
        self._bar_set = ()

    def _add(self, eng, fn, r, w, is_dma=False):
        op = _Op(len(self.ops), eng, fn, is_dma)
        deps = set()
        if self._bar_pending.get(eng):
            self._bar_pending[eng] = False
            deps |= self._bar_set
        for k in r:
            if k in self.lastw:
                deps.add(self.lastw[k])
        for k in w:
            if k in self.lastw:
                deps.add(self.lastw[k])
            deps |= self.readers.get(k, set())
        for k in r:
            self.readers.setdefault(k, set()).add(op.id)
        for k in w:
            self.lastw[k] = op.id
            self.readers[k] = set()
        if is_dma:
            slot = (eng, self.dma_n[eng] % NRING)
            self.dma_n[eng] += 1
            op.ring = slot
            self.ring_cnt[slot] = self.ring_cnt.get(slot, 0) + 1
            op.ringval = 16 * self.ring_cnt[slot]
            if slot in self.ring_last:
                deps.add(self.ring_last[slot])
            self.ring_last[slot] = op.id
        deps.discard(op.id)
        op.deps = deps
        op.pos = len(self.eng_ops[eng])
        self.eng_ops[eng].append(op)
        self.ops.append(op)
        return op

    def barrier(self):
        s = set()
        for e in ENGS:
            if self.eng_ops[e]:
                s.add(self.eng_ops[e][-1].id)
        for slot, oid in self.ring_last.items():
            s.add(oid)
        self._bar_set = s
        self._bar_pending = {e: True for e in ENGS}
        self.lastw = {}
        self.readers = {}

    def pe(self, fn, r=(), w=()):
        return self._add("pe", fn, r, w)

    def act(self, fn, r=(), w=()):
        return self._add("act", fn, r, w)

    def dve(self, fn, r=(), w=()):
        return self._add("dve", fn, r, w)

    def pool(self, fn, r=(), w=()):
        return self._add("pool", fn, r, w)

    def dma(self, fn, r=(), w=(), q="sp"):
        return self._add(q, fn, r, w, is_dma=True)

    def emit(self, nc):
        ops = self.ops
        fin = set()
        for e in ENGS:
            if self.eng_ops[e]:
                fin.add(self.eng_ops[e][-1].id)
        for slot, oid in self.ring_last.items():
            fin.add(oid)
        for op in ops:
            nd = set()
            for d in op.deps:
                dop = ops[d]
                if dop.eng == op.eng and not dop.is_dma and not op.is_dma:
                    if op.eng == "pe":
                        continue
                    if op.pos - dop.pos > 1:
                        continue
                nd.add(d)
            op.deps = nd
        for op in ops:
            for d in op.deps:
                ops[d].marked = True
        for d in fin:
            ops[d].marked = True
        for e in ENGS:
            c = 0
            for op in self.eng_ops[e]:
                if op.is_dma:
                    continue
                if op.marked:
                    c += 1
                    op.val = c
        with ExitStack() as st:
            sems = {e: st.enter_context(nc.semaphore("s_" + e)) for e in ENGS}
            rings = {}
            for slot in self.ring_cnt:
                rings[slot] = st.enter_context(nc.semaphore("r_%s_%d" % slot))
            block = st.enter_context(nc.Block())

            def token(op):
                if op.is_dma:
                    return (rings[op.ring], op.ringval, ("r",) + op.ring)
                return (sems[op.eng], op.val, ("e", op.eng))

            def run(e, eng):
                waited = {}
                for op in self.eng_ops[e]:
                    need = {}
                    for d in op.deps:
                        s, v, key = token(ops[d])
                        if waited.get(key, 0) >= v:
                            continue
                        if need.get(key, (None, 0))[1] < v:
                            need[key] = (s, v)
                    for key, (s, v) in need.items():
                        eng.wait_ge(s, v)
                        waited[key] = v
                    ins = op.fn(eng)
                    if op.is_dma:
                        ins.then_inc(rings[op.ring], 16)
                    elif op.marked:
                        ins.then_inc(sems[e], 1)
                if e == "sp":
                    for d in sorted(fin):
                        s, v, key = token(ops[d])
                        if waited.get(key, 0) >= v:
                            continue
                        eng.wait_ge(s, v)
                        waited[key] = v

            block.tensor(lambda eng: run("pe", eng))
            block.scalar(lambda eng: run("act", eng))
            block.vector(lambda eng: run("dve", eng))
            block.gpsimd(lambda eng: run("pool", eng))
            block.sync(lambda eng: run("sp", eng))


def I(name, *a, **kw):
    return lambda e: getattr(e, name)(*a, **kw)


def G(calls):
    calls = list(calls)

    def fn(e):
        ins = None
        for (name, kw) in calls:
            ins = getattr(e, name)(**kw)
        return ins
    return fn


def MM(out, lhsT, rhs, start=True, stop=True, skip=False):
    kw = dict(out=out, lhsT=lhsT, rhs=rhs, start=start, stop=stop)
    if skip:
        kw["skip_group_check"] = True
    return ("matmul", kw)


class _Stop(Exception):
    pass


class Arena:
    def __init__(self, ap, nwords):
        self.ar = ap
        self.n = nwords
        self.top = 0

    def mark(self):
        return self.top

    def release(self, m):
        self.top = m

    def alloc(self, shape, dt):
        nelem = 1
        for s in shape:
            nelem *= s
        nb = 2 if dt == BF16 else 4
        words = (nelem * nb + 3) // 4
        off = self.top
        self.top += words
        assert self.top <= self.n, "arena overflow %d > %d" % (self.top, self.n)
        v = self.ar[:, off:off + words]
        if dt != F32:
            v = v.bitcast(dt)[:, 0:nelem]
        if len(shape) == 2:
            v = v.rearrange("p (a b) -> p a b", a=shape[0])
        elif len(shape) == 3:
            v = v.rearrange("p (a b c) -> p a b c", a=shape[0], b=shape[1])
        return v


def a_ext_cols():
    cols = []
    q0, k0, v0, qi0, ki0, wi0 = 0, 1024, 2048, 3072, 3584, 3648

    def perm(base, width):
        half = width // 2
        return [base + (i + half) % width for i in range(width)]
    for h in range(8):
        cols += list(range(q0 + h * 128, q0 + (h + 1) * 128))
    for h in range(8):
        cols += perm(q0 + h * 128, 128)
    for h in range(8):
        cols += list(range(k0 + h * 128, k0 + (h + 1) * 128))
    for h in range(8):
        cols += perm(k0 + h * 128, 128)
    for h in range(8):
        cols += list(range(qi0 + h * 64, qi0 + (h + 1) * 64))
    for h in range(8):
        cols += perm(qi0 + h * 64, 64)
    cols += list(range(ki0, ki0 + 64)) * 2
    cols += perm(ki0, 64) * 2
    cols += list(range(v0, v0 + 1024))
    cols += list(range(wi0, wi0 + 8))
    return np.array(cols, dtype=np.int64)


BLK_Q, BLK_QP, BLK_K, BLK_KP, BLK_QI, BLK_QIP, BLK_KI, BLK_KIP = 0, 8, 16, 24, 32, 36, 40, 41
COL_V = 42 * 128
COL_WI = COL_V + 1024
NCOL_EXT = COL_WI + 8


def build(S, NSEQ, TOPK, stop_after=None, debug=False):
    NT = NSEQ * S
    NTL = S // 128
    NG = S // 512
    assert S % 512 == 0
    nc = bass.Bass("TRN2", target_bir_lowering=False)

    def din(name, shape):
        return nc.dram_tensor(name, shape, F32, kind="ExternalInput").ap()
    x = din("x", [NT, D])
    w_in = din("w_in_ext", [D, NCOL_EXT])
    a_w_out = din("a_w_out", [D, D])
    b_w_q = din("b_w_q", [D, D])
    b_w_kv = din("b_w_kv", [D, 2 * D])
    b_w_out = din("b_w_out", [D, D])
    router_w = din("router_w", [D, NE])
    router_b = din("router_bias", [1, NE])
    w_gate = din("exp_w_gate", [2, NE, D, DFF])
    w_up = din("exp_w_up", [2, NE, D, DFF])
    w_down = din("exp_w_down", [2, NE, DFF, D])
    ln_g = din("ln_g", [4, D])
    ln_b = din("ln_b", [4, D])
    c_ident = din("c_ident", [128, 128])
    c_tri = din("c_tri", [128, 128])
    c_caus = din("c_caus", [128, 128])
    c_rope = din("c_rope", [4, 128, S])
    out = nc.dram_tensor("out", [NT, D], F32, kind="ExternalOutput").ap()
    skind = "ExternalOutput" if debug else "Internal"
    h1 = nc.dram_tensor("h1", [NT, D], F32, kind=skind).ap()
    h2 = nc.dram_tensor("h2", [NT, D], F32, kind=skind).ap()
    h3 = nc.dram_tensor("h3", [NT, D], F32, kind=skind).ap()
    qT_d = nc.dram_tensor("qT_d", [8, 128, S], BF16, kind="Internal").ap()
    qiT_d = nc.dram_tensor("qiT_d", [4, 128, S], BF16, kind="Internal").ap()

    P = Prog()
    es = ExitStack()
    ARW = 51800
    arena_t = es.enter_context(nc.sbuf_tensor("arena", [128, ARW], F32))
    ar = Arena(arena_t, ARW)
    psum_t = es.enter_context(nc.psum_tensor("psum", [128, 8, 512], F32))
    PS = [psum_t[:, b, :] for b in range(8)]
    PSB = [psum_t[:, b, :].bitcast(BF16) for b in range(8)]

    def psk(b):
        return ("ps", b)

    identF = ar.alloc([128], F32)
    identB = ar.alloc([128], BF16)
    onesB = ar.alloc([128], BF16)
    onesF = ar.alloc([128], F32)
    triB = ar.alloc([128], BF16)
    causF = ar.alloc([128], F32)
    u2B = ar.alloc([128], BF16)
    ctmp = ar.alloc([128], F32)
    rw32 = ar.alloc([8, NE], F32)
    rbias = ar.alloc([NE], F32)
    pw = ar.alloc([NIT], F32)
    sel8 = ar.alloc([4, 8], F32)
    epsT = ar.alloc([1], F32)
    P.dma(I("dma_start", out=identF, in_=c_ident), w=["identF"])
    P.dve(I("tensor_copy", out=identB, in_=identF), r=["identF"], w=["identB"])
    P.dve(I("memset", onesB, 1.0), w=["onesB"])
    P.dve(I("memset", onesF, 1.0), w=["onesF"])
    P.dma(I("dma_start", out=ctmp, in_=c_tri), w=["ctmp"])
    P.dve(I("tensor_copy", out=triB, in_=ctmp), r=["ctmp"], w=["triB"])
    P.dma(I("dma_start", out=causF, in_=c_caus), w=["causF"])
    P.dve(I("tensor_copy", out=u2B, in_=causF), r=["causF"], w=["u2B"])
    P.dma(I("dma_start", out=rw32, in_=router_w.rearrange("(k p) e -> p k e", p=128)), w=["rw32"])
    P.dma(I("dma_start", out=rbias, in_=router_b.broadcast_to([128, NE])), w=["rbias"])
    for it in range(NIT):
        P.dve(I("memset", pw[:, it:it + 1], 2.0 ** -(it + 1)), w=["pw"])
    P.dve(I("memset", sel8, -1e30), w=["sel8"])
    P.dve(I("memset", epsT, LN_EPS), w=["epsT"])
    base_mark = ar.mark()
    P.barrier()

    def layer_norm(pre, kpre, yo, kyo, gb, small, kidx):
        bst, mv, lnv, rstd, nmr = small
        ks = ("lnsm", kidx)
        P.dve(I("bn_stats", out=bst[:, 0, :], in_=pre[:, 0:512]), r=[kpre], w=[ks])
        P.dve(I("bn_stats", out=bst[:, 1, :], in_=pre[:, 512:1024]), r=[kpre], w=[ks])
        P.dve(I("bn_aggr", out=mv, in_=bst), r=[ks], w=[ks])
        P.act(I("activation", out=lnv, in_=mv[:, 1:2], func=AF.Ln, bias=epsT[:, 0:1], scale=1.0), r=[ks, "epsT"], w=[ks])
        P.act(I("activation", out=rstd, in_=lnv, func=AF.Exp, scale=-0.5), r=[ks], w=[ks])
        P.dve(I("tensor_scalar", out=nmr, in0=mv[:, 0:1], scalar1=rstd[:, 0:1], scalar2=-1.0, op0=ALU.mult, op1=ALU.mult), r=[ks], w=[ks])
        P.act(I("activation", out=pre, in_=pre, func=AF.Identity, scale=rstd[:, 0:1], bias=nmr[:, 0:1]), r=[ks], w=[kpre])
        P.pool(I("tensor_tensor", out=pre, in0=pre, in1=gb[:, 0, :], op=ALU.mult), r=["gb"], w=[kpre])
        P.pool(I("tensor_tensor", out=yo, in0=pre, in1=gb[:, 1, :], op=ALU.add), r=["gb", kpre], w=[kyo])

    def ln_smalls():
        return (ar.alloc([2, 6], F32), ar.alloc([2], F32), ar.alloc([1], F32), ar.alloc([1], F32), ar.alloc([1], F32))

    def load_gb(gb, idx):
        P.dma(I("dma_start", out=gb[:, 0, :], in_=ln_g[idx:idx + 1, :].broadcast_to([128, D])), w=["gb"])
        P.dma(I("dma_start", out=gb[:, 1, :], in_=ln_b[idx:idx + 1, :].broadcast_to([128, D])), w=["gb"])

    def build_xT(src, tok0, BA, xs):
        for i in range(NTL):
            xb = xs[i % len(xs)]
            kx = ("xs", i % len(xs))
            P.dma(I("dma_start", out=xb, in_=src[tok0 + i * 128: tok0 + (i + 1) * 128, :]), w=[kx])
            ba, bb = (0, 1) if i % 2 == 0 else (2, 3)
            calls = []
            for k in range(8):
                bank = ba if k < 4 else bb
                calls.append(("transpose", dict(out=PS[bank][:, (k % 4) * 128:(k % 4 + 1) * 128], in_=xb[:, k * 128:(k + 1) * 128], identity=identF)))
            P.pe(G(calls), r=[kx, "identF"], w=[psk(ba), psk(bb)])
            P.act(I("copy", out=BA[:, 0:4, i * 128:(i + 1) * 128], in_=PS[ba].rearrange("p (a b) -> p a b", a=4)), w=[psk(ba), ("BA", i, 0)])
            P.dve(I("tensor_copy", out=BA[:, 4:8, i * 128:(i + 1) * 128], in_=PS[bb].rearrange("p (a b) -> p a b", a=4)), w=[psk(bb), ("BA", i, 1)])

    def ba_keys(t0, t1):
        ks = []
        for i in range(t0 // 128, (t1 + 127) // 128):
            ks += [("BA", i, 0), ("BA", i, 1)]
        return ks

    def load_wcol(dst, key, wap, c0):
        P.dma(I("dma_start", out=dst, in_=wap[:, c0:c0 + 128].rearrange("(k p) c -> p k c", p=128)), w=[key], q="pool")

    def out_proj_ln(BA, WBIG, src, dst, tok0, lnidx, xs, pres, gb, smalls):
        NX, NP_ = len(xs), len(pres)

        def s0(i):
            xb, kx = xs[i % NX], ("xs", i % NX)
            pre, kp = pres[i % NP_], ("pre", i % NP_)
            bst, mv, lnv, rstd, nmr = smalls[i % NP_]
            ks = ("lnsm", i % NP_)
            P.dma(I("dma_start", out=xb, in_=src[tok0 + i * 128: tok0 + (i + 1) * 128, :]), w=[kx])
            for c in range(2):
                bank = (i % 2) * 2 + c
                P.pe(G([MM(PS[bank], BA[:, k, i * 128:(i + 1) * 128], WBIG[:, k, c * 512:(c + 1) * 512], start=(k == 0), stop=(k == 7)) for k in range(8)]),
                     r=["oT", "WBIG"], w=[psk(bank)])
                P.dve(I("scalar_tensor_tensor", out=pre[:, c * 512:(c + 1) * 512], in0=xb[:, c * 512:(c + 1) * 512], scalar=ALPHA, in1=PS[bank], op0=ALU.mult, op1=ALU.add),
                      r=[kx], w=[psk(bank), (kp, c)])
                P.dve(I("bn_stats", out=bst[:, c, :], in_=pre[:, c * 512:(c + 1) * 512]), r=[(kp, c)], w=[(ks, "b", c)])
            P.dve(I("bn_aggr", out=mv, in_=bst), r=[(ks, "b", 0), (ks, "b", 1)], w=[(ks, "mv")])

        def s1(i):
            bst, mv, lnv, rstd, nmr = smalls[i % NP_]
            ks = ("lnsm", i % NP_)
            P.act(I("activation", out=lnv, in_=mv[:, 1:2], func=AF.Ln, bias=epsT[:, 0:1], scale=1.0), r=[(ks, "mv"), "epsT"], w=[(ks, "ln")])
            P.act(I("activation", out=rstd, in_=lnv, func=AF.Exp, scale=-0.5), r=[(ks, "ln")], w=[(ks, "rstd")])

        def s2(i):
            pre, kp = pres[i % NP_], ("pre", i % NP_)
            bst, mv, lnv, rstd, nmr = smalls[i % NP_]
            ks = ("lnsm", i % NP_)
            P.dve(I("tensor_scalar", out=nmr, in0=mv[:, 0:1], scalar1=rstd[:, 0:1], scalar2=-1.0, op0=ALU.mult, op1=ALU.mult), r=[(ks, "mv"), (ks, "rstd")], w=[(ks, "nmr")])
            P.act(I("activation", out=pre, in_=pre, func=AF.Identity, scale=rstd[:, 0:1], bias=nmr[:, 0:1]), r=[(ks, "rstd"), (ks, "nmr")], w=[(kp, 0), (kp, 1)])

        def s3(i):
            pre, kp = pres[i % NP_], ("pre", i % NP_)
            P.pool(I("tensor_tensor", out=pre, in0=pre, in1=gb[:, 0, :], op=ALU.mult), r=["gb"], w=[(kp, 0), (kp, 1)])
            P.pool(I("tensor_tensor", out=pre, in0=pre, in1=gb[:, 1, :], op=ALU.add), r=["gb"], w=[(kp, 0), (kp, 1)])
            P.dma(I("dma_start", out=dst[tok0 + i * 128: tok0 + (i + 1) * 128, :], in_=pre), r=[(kp, 0), (kp, 1)], w=[("dst", i)])

        for n in range(NTL + 3):
            if n < NTL:
                s0(n)
            if 0 <= n - 1 < NTL:
                s1(n - 1)
            if 0 <= n - 2 < NTL:
                s2(n - 2)
            if 0 <= n - 3 < NTL:
                s3(n - 3)

    def mixer_A(b):
        tok0 = b * S
        ar.release(base_mark)
        BK = ar.alloc([8, S], BF16)
        BV = ar.alloc([NTL, D], BF16)
        BKI = ar.alloc([S], BF16)
        BA = ar.alloc([8, S], BF16)
        wiall = ar.alloc([NTL, 8], F32)
        aw = ar.alloc([NTL, 8], F32)
        sg = ar.alloc([NTL, 8], F32)
        m2 = ar.mark()
        WBIG = ar.alloc([8, D], BF16)
        rope = ar.alloc([2, S], F32)
        WC = [ar.alloc([8, 128], BF16) for _ in range(4)]
        Wwi = ar.alloc([8, 8], BF16)
        xs = [ar.alloc([D], F32) for _ in range(2)]
        t1 = [ar.alloc([512], F32) for _ in range(2)]
        t2 = [ar.alloc([512], F32) for _ in range(2)]
        ob = [ar.alloc([512], BF16) for _ in range(2)]

        build_xT(x, tok0, BA, xs)
        P.dma(I("dma_start", out=WBIG, in_=w_in[:, COL_V:COL_V + D].rearrange("(k p) c -> p k c", p=128)), w=["WBIG"], q="pool")
        P.dma(I("dma_start", out=Wwi, in_=w_in[:, COL_WI:COL_WI + 8].rearrange("(k p) c -> p k c", p=128)), w=["Wwi"], q="pool")

        blocks = []
        for j in range(4):
            blocks.append((BLK_QI + j, BLK_QIP + j, 1, ("qi", j)))
        blocks.append((BLK_KI, BLK_KIP, 1, ("ki", 0)))
        for h in range(8):
            blocks.append((BLK_Q + h, BLK_QP + h, 0, ("q", h)))
        for h in range(8):
            blocks.append((BLK_K + h, BLK_KP + h, 0, ("k", h)))
        cur_fam = None
        n = 0
        for bi, (ba_, bp_, fam, dest) in enumerate(blocks):
            if fam != cur_fam:
                cur_fam = fam
                t_c, t_s = (2, 3) if fam == 1 else (0, 1)
                P.dma(I("dma_start", out=rope[:, 0, :], in_=c_rope[t_c]), w=["rope"])
                P.dma(I("dma_start", out=rope[:, 1, :], in_=c_rope[t_s]), w=["rope"])
            wa, wp = WC[2 * (bi % 2)], WC[2 * (bi % 2) + 1]
            ka, kp_ = ("WC", 2 * (bi % 2)), ("WC", 2 * (bi % 2) + 1)
            if bi == 0:
                load_wcol(wa, ka, w_in, ba_ * 128)
                load_wcol(wp, kp_, w_in, bp_ * 128)
            if bi + 1 < len(blocks):
                nb1 = (bi + 1) % 2
                load_wcol(WC[2 * nb1], ("WC", 2 * nb1), w_in, blocks[bi + 1][0] * 128)
                load_wcol(WC[2 * nb1 + 1], ("WC", 2 * nb1 + 1), w_in, blocks[bi + 1][1] * 128)
            for tg in range(NG):
                c0, c1 = tg * 512, (tg + 1) * 512
                pa, pb = (0, 1) if n % 2 == 0 else (2, 3)
                j2 = n % 2
                n += 1
                P.pe(G([MM(PS[pa], wa[:, k, :], BA[:, k, c0:c1], start=(k == 0), stop=(k == 7)) for k in range(8)]), r=[ka] + ba_keys(c0, c1), w=[psk(pa)])
                P.pe(G([MM(PS[pb], wp[:, k, :], BA[:, k, c0:c1], start=(k == 0), stop=(k == 7)) for k in range(8)]), r=[kp_] + ba_keys(c0, c1), w=[psk(pb)])
                P.dve(I("tensor_tensor", out=t1[j2], in0=PS[pa], in1=rope[:, 0, c0:c1], op=ALU.mult), r=["rope"], w=[psk(pa), ("t1", j2)])
                P.dve(I("tensor_tensor", out=t2[j2], in0=PS[pb], in1=rope[:, 1, c0:c1], op=ALU.mult), r=["rope"], w=[psk(pb), ("t2", j2)])
                kind, idx = dest
                if kind == "k":
                    P.pool(I("tensor_tensor", out=BK[:, idx, c0:c1], in0=t1[j2], in1=t2[j2], op=ALU.add), r=[("t1", j2), ("t2", j2)], w=[("BK", idx)])
                elif kind == "ki":
                    P.pool(I("tensor_tensor", out=BKI[:, c0:c1], in0=t1[j2], in1=t2[j2], op=ALU.add), r=[("t1", j2), ("t2", j2)], w=["BKI"])
                else:
                    P.pool(I("tensor_tensor", out=ob[j2], in0=t1[j2], in1=t2[j2], op=ALU.add), r=[("t1", j2), ("t2", j2)], w=[("ob", j2)])
                    dd = qT_d if kind == "q" else qiT_d
                    P.dma(I("dma_start", out=dd[idx, :, c0:c1], in_=ob[j2]), r=[("ob", j2)], w=[(kind + "d", tg)])
        if stop_after == "A.1":
            raise _Stop()
        for i in range(NTL):
            for c in range(2):
                bank = 4 + (2 * i + c) % 4
                P.pe(G([MM(PS[bank], BA[:, k, i * 128:(i + 1) * 128], WBIG[:, k, c * 512:(c + 1) * 512], start=(k == 0), stop=(k == 7)) for k in range(8)]),
                     r=ba_keys(i * 128, (i + 1) * 128) + ["WBIG"], w=[psk(bank)])
                if c == 0:
                    P.act(I("copy", out=BV[:, i, 0:512], in_=PS[bank]), w=[psk(bank), ("BV", i)])
                else:
                    P.dve(I("tensor_copy", out=BV[:, i, 512:1024], in_=PS[bank]), w=[psk(bank), ("BV", i)])
            bank = i % 2
            P.pe(G([MM(PS[bank][:, 0:8], BA[:, k, i * 128:(i + 1) * 128], Wwi[:, k, :], start=(k == 0), stop=(k == 7)) for k in range(8)]),
                 r=ba_keys(i * 128, (i + 1) * 128) + ["Wwi"], w=[psk(bank)])
            P.dve(I("tensor_copy", out=wiall[:, i, :], in_=PS[bank][:, 0:8]), w=[psk(bank), "wiall"])
        P.dve(I("tensor_scalar", out=sg, in0=wiall, scalar1=-IDX_C, scalar2=None, op0=ALU.mult), r=["wiall"], w=["sg"])
        P.dve(I("scalar_tensor_tensor", out=aw, in0=wiall, scalar=IDX_C, in1=sg, op0=ALU.mult, op1=ALU.max), r=["wiall", "sg"], w=["aw"])
        P.dve(I("tensor_scalar", out=sg, in0=wiall, scalar1=0.0, scalar2=2.0, op0=ALU.is_ge, op1=ALU.mult), r=["wiall"], w=["sg"])
        P.dve(I("tensor_scalar", out=sg, in0=sg, scalar1=-1.0, scalar2=None, op0=ALU.add), w=["sg"])

        if stop_after == "A.2":
            raise _Stop()
        P.barrier()
        ar.release(m2)
        qg = [ar.alloc([8, 512], BF16) for _ in range(2)]
        qig = [ar.alloc([4, 512], BF16) for _ in range(2)]
        maskT = [ar.alloc([NTL, 512], BF16) for _ in range(2)]
        score = [ar.alloc([S], F32) for _ in range(2)]
        MB = [ar.alloc([S], BF16) for _ in range(2)]
        Rt = [ar.alloc([512], BF16) for _ in range(4)]
        Dg = [ar.alloc([8, 128], BF16) for _ in range(2)]
        PT = [ar.alloc([512], BF16) for _ in range(3)]
        U = ar.alloc([512], F32)
        Vs = ar.alloc([512], F32)
        bsm = [dict(rmax=ar.alloc([1], F32), rmin=ar.alloc([1], F32), rng=ar.alloc([1], F32), dtab=ar.alloc([NIT], F32),
                    cand=ar.alloc([1], F32), cnt=ar.alloc([1], F32), step=ar.alloc([1], F32), cur=ar.alloc([1], F32)) for _ in range(2)]
        cnts = {"R": 0, "S": 0, "P": 0, "Rt": 0}

        def load_group(g):
            P.dma(I("dma_start", out=qg[g % 2], in_=qT_d[:, :, g * 512:(g + 1) * 512].rearrange("h p s -> p h s")), w=[("qg", g % 2)])
            P.dma(I("dma_start", out=qig[g % 2], in_=qiT_d[:, :, g * 512:(g + 1) * 512].rearrange("h p s -> p h s")), w=[("qig", g % 2)])

        def idx_scores(g, j):
            qigb = qig[g % 2]
            i = 4 * g + j
            L1, L2 = 128 * i + 64, 128 * i + 128
            sc = score[j % 2]
            ksc = ("score", j % 2)
            dg = Dg[j % 2]
            kdg = ("Dg", j % 2)
            P.dve(I("tensor_tensor", out=dg, in0=identB.unsqueeze(1).to_broadcast([128, 8, 128]), in1=sg[:, i, :].unsqueeze(2).to_broadcast([128, 8, 128]), op=ALU.mult),
                  r=["identB", "sg"], w=[kdg])
            steps = []
            for kr in range(0, L2, 512):
                for h in range(8):
                    steps.append((kr, min(512, L2 - kr), h))
            info = {}

            def rel(n):
                kr, w_, h = steps[n]
                hp = (h % 2) * 64
                bank = cnts["R"] % 2
                cnts["R"] += 1
                info[n] = bank
                P.pe(I("matmul", out=PS[bank][:, 0:w_], lhsT=qigb[hp:hp + 64, h // 2, j * 128:(j + 1) * 128], rhs=BKI[hp:hp + 64, kr:kr + w_], start=True, stop=True),
                     r=[("qig", g % 2), "BKI"], w=[psk(bank)])

            def relu_acc(n):
                kr, w_, h = steps[n]
                bank = info[n]
                rt = Rt[cnts["Rt"] % 4]
                krt = ("Rt", cnts["Rt"] % 4)
                cnts["Rt"] += 1
                P.act(I("activation", out=rt[:, 0:w_], in_=PS[bank][:, 0:w_], func=AF.Relu, scale=aw[:, i, h:h + 1]), r=["aw"], w=[psk(bank), krt])
                P.pe(I("matmul", out=PS[7][:, 0:w_], lhsT=dg[:, h, :], rhs=rt[:, 0:w_], start=(h == 0), stop=(h == 7)), r=[krt, kdg], w=[psk(7)])
                if h == 7:
                    P.dve(I("tensor_copy", out=sc[:, kr:kr + w_], in_=PS[7][:, 0:w_]), w=[psk(7), ksc])
            rel(0)
            for n in range(len(steps)):
                if n + 1 < len(steps):
                    rel(n + 1)
                relu_acc(n)
            P.dve(I("memset", sc[0:64, L1:L2], -3.0e38), w=[ksc])

        def bis_ops(g, j):
            i = 4 * g + j
            L1, L2 = 128 * i + 64, 128 * i + 128
            sc, mb, sm = score[j % 2], MB[j % 2], bsm[j % 2]
            ksc, kb_, kmb, kcur = ("score", j % 2), ("bis", j % 2), ("MB", j % 2), ("cur", j % 2)
            ops = []
            if L1 <= TOPK:
                ops.append(lambda: P.dve(I("memset", sm["cur"], -1.0e30), w=[kcur]))
                return ops
            ops.append(lambda: P.dve(I("tensor_reduce", out=sm["rmax"], in_=sc[:, 0:L2], axis=AX.X, op=ALU.max), r=[ksc], w=[kb_]))
            ops.append(lambda: P.dve(I("tensor_reduce", out=sm["rmin"], in_=sc[:, 0:L1], axis=AX.X, op=ALU.min), r=[ksc], w=[kb_]))
            ops.append(lambda: P.dve(I("tensor_tensor", out=sm["rng"], in0=sm["rmax"], in1=sm["rmin"], op=ALU.subtract), w=[kb_]))
            ops.append(lambda: P.dve(I("tensor_scalar", out=sm["dtab"], in0=pw, scalar1=sm["rng"][:, 0:1], scalar2=None, op0=ALU.mult), r=["pw"], w=[kb_]))
            ops.append(lambda: P.dve(I("tensor_tensor", out=sm["cand"], in0=sm["rmin"], in1=sm["dtab"][:, 0:1], op=ALU.add), w=[kb_]))
            for it in range(NIT):
                ops.append(lambda: P.dve(I("tensor_scalar", out=mb[:, 0:L2], in0=sc[:, 0:L2], scalar1=sm["cand"][:, 0:1], scalar2=0.0, op0=ALU.is_ge, op1=ALU.add, accum_out=sm["cnt"]),
                                         r=[ksc], w=[kb_, kmb]))
                ops.append(lambda it=it: P.dve(I("tensor_scalar", out=sm["step"], in0=sm["cnt"], scalar1=float(TOPK) - 0.5, scalar2=sm["dtab"][:, it:it + 1], op0=ALU.is_ge, op1=ALU.mult), w=[kb_]))
                if it < NIT - 1:
                    ops.append(lambda it=it: P.dve(I("scalar_tensor_tensor", out=sm["cand"], in0=sm["step"], scalar=sm["dtab"][:, it + 1:it + 2], in1=sm["cand"], op0=ALU.subtract, op1=ALU.add), w=[kb_]))
                else:
                    ops.append(lambda it=it: P.dve(I("scalar_tensor_tensor", out=sm["cur"], in0=sm["step"], scalar=sm["dtab"][:, it:it + 1], in1=sm["cand"], op0=ALU.subtract, op1=ALU.add), w=[kb_, kcur]))
            return ops

        def mask_tile(g, j):
            i = 4 * g + j
            L2 = 128 * i + 128
            sc, mb, sm = score[j % 2], MB[j % 2], bsm[j % 2]
            mT = maskT[g % 2]
            P.dve(I("tensor_scalar", out=mb[:, 0:L2], in0=sc[:, 0:L2], scalar1=sm["cur"][:, 0:1], scalar2=NEG, op0=ALU.is_lt, op1=ALU.mult),
                  r=[("score", j % 2), ("cur", j % 2)], w=[("MB", j % 2)])
            for kb0 in range(0, i + 1, 4):
                nb = min(4, i + 1 - kb0)
                bank = cnts["R"] % 2
                cnts["R"] += 1
                P.pe(G([("transpose", dict(out=PSB[bank][:, q * 128:(q + 1) * 128], in_=mb[:, (kb0 + q) * 128:(kb0 + q + 1) * 128], identity=identB)) for q in range(nb)]),
                     r=[("MB", j % 2), "identB"], w=[psk(bank)])
                P.act(I("copy", out=mT[:, kb0:kb0 + nb, j * 128:(j + 1) * 128], in_=PSB[bank][:, 0:nb * 128].rearrange("p (a b) -> p a b", a=nb)),
                      w=[psk(bank), ("maskT", g % 2, j)])

        def att_heads(g, heads):
            qgb = qg[g % 2]
            mT = maskT[g % 2]
            nkb = 4 * g + 4
            bo, bs = 5, 6
            blks = [(h, kb) for h in heads for kb in range(nkb)]
            info = {}

            def sS(n):
                h, kb = blks[n]
                jl = max(0, kb - 4 * g)
                q0 = jl * 128
                N = 512 - q0
                sb_ = 2 + (cnts["S"] % 3)
                cnts["S"] += 1
                info[n] = sb_
                P.pe(G([MM(PS[sb_][:, 0:N], BK[:, h, kb * 128:(kb + 1) * 128], qgb[:, h, q0:512], start=True, stop=False),
                        MM(PS[sb_][:, 0:N], identB, mT[:, kb, q0:512], start=False, stop=True)]),
                     r=[("BK", h), ("qg", g % 2), "identB"] + [("maskT", g % 2, jj) for jj in range(jl, 4)], w=[psk(sb_)])

            def sPV(n):
                h, kb = blks[n]
                jl = max(0, kb - 4 * g)
                q0 = jl * 128
                N = 512 - q0
                sb_ = info[n]
                pt = PT[cnts["P"] % 3]
                kpt = ("PT", cnts["P"] % 3)
                cnts["P"] += 1
                P.act(I("activation", out=pt[:, 0:N], in_=PS[sb_][:, 0:N], func=AF.Exp, scale=QK_SCALE), w=[psk(sb_), kpt])
                P.pe(G([MM(PS[bo][:, q0:512], BV[:, kb, h * 128:(h + 1) * 128], pt[:, 0:N], start=(kb == 0), stop=(kb == nkb - 1)),
                        MM(PS[bs][:, q0:512], onesB, pt[:, 0:N], start=(kb == 0), stop=(kb == nkb - 1))]),
                     r=[kpt, ("BV", kb), "onesB"], w=[psk(bo), psk(bs)])
                if kb == nkb - 1:
                    P.act(I("copy", out=U, in_=PS[bo]), w=[psk(bo), "U"])
                    P.act(I("activation", out=Vs, in_=PS[bs], func=AF.Ln), w=[psk(bs), "Vs"])
                    P.act(I("activation", out=Vs, in_=Vs, func=AF.Exp, scale=-1.0), w=["Vs"])
                    P.pool(I("tensor_tensor", out=BA[:, h, g * 512:(g + 1) * 512], in0=U, in1=Vs, op=ALU.mult), r=["U", "Vs"], w=["oT"])
            sS(0)
            for n in range(len(blks)):
                if n + 1 < len(blks):
                    sS(n + 1)
                sPV(n)

        for g in range(NG + 1):
            if g < NG:
                load_group(g)
            for pr in range(2):
                if g < NG:
                    idx_scores(g, 2 * pr)
                    idx_scores(g, 2 * pr + 1)
                    la, lb = bis_ops(g, 2 * pr), bis_ops(g, 2 * pr + 1)
                    for k in range(max(len(la), len(lb))):
                        if k < len(la):
                            la[k]()
                        if k < len(lb):
                            lb[k]()
                if g >= 1:
                    att_heads(g - 1, list(range(4 * pr, 4 * pr + 4)))
                if g < NG:
                    mask_tile(g, 2 * pr)
                    mask_tile(g, 2 * pr + 1)
        if stop_after == "A.3":
            raise _Stop()
        P.barrier()
        ar.release(m2)
        WBIG = ar.alloc([8, D], BF16)
        P.dma(I("dma_start", out=WBIG, in_=a_w_out.rearrange("(k p) c -> p k c", p=128)), w=["WBIG"], q="pool")
        gb = ar.alloc([2, D], F32)
        load_gb(gb, 0)
        xs2 = [ar.alloc([D], F32) for _ in range(3)]
        pres = [ar.alloc([D], F32) for _ in range(5)]
        smalls = [ln_smalls() for _ in range(5)]
        out_proj_ln(BA, WBIG, x, h1, tok0, 0, xs2, pres, gb, smalls)
        P.barrier()

    def mixer_B(b):
        tok0 = b * S
        ar.release(base_mark)
        BK = ar.alloc([8, S], BF16)
        BQ = ar.alloc([8, S], BF16)
        BV = ar.alloc([NTL, D], BF16)
        BA = ar.alloc([8, S], BF16)
        WBIG = ar.alloc([8, D], BF16)
        m2 = ar.mark()
        WC = [ar.alloc([8, 128], BF16) for _ in range(2)]
        xs = [ar.alloc([D], F32) for _ in range(3)]
        build_xT(h2, tok0, BA, xs)
        P.dma(I("dma_start", out=WBIG, in_=b_w_kv[:, D:2 * D].rearrange("(k p) c -> p k c", p=128)), w=["WBIG"], q="pool")
        n = 0
        for bi in range(16):
            wap, c0w, dstB, kd = (b_w_q, bi * 128, BQ, ("BQ", bi)) if bi < 8 else (b_w_kv, (bi - 8) * 128, BK, ("BK", bi - 8))
            wa, ka = WC[bi % 2], ("WC", bi % 2)
            if bi == 0:
                load_wcol(wa, ka, wap, c0w)
            if bi + 1 < 16:
                bn = bi + 1
                wapn, c0n = (b_w_q, bn * 128) if bn < 8 else (b_w_kv, (bn - 8) * 128)
                load_wcol(WC[bn % 2], ("WC", bn % 2), wapn, c0n)
            for tg in range(NG):
                c0, c1 = tg * 512, (tg + 1) * 512
                pa = n % 4
                n += 1
                P.pe(G([MM(PS[pa], wa[:, k, :], BA[:, k, c0:c1], start=(k == 0), stop=(k == 7)) for k in range(8)]), r=[ka] + ba_keys(c0, c1), w=[psk(pa)])
                if n % 2 == 0:
                    P.act(I("copy", out=dstB[:, bi % 8, c0:c1], in_=PS[pa]), w=[psk(pa), kd])
                else:
                    P.dve(I("tensor_copy", out=dstB[:, bi % 8, c0:c1], in_=PS[pa]), w=[psk(pa), kd])
        for i in range(NTL):
            for c in range(2):
                bank = 4 + (2 * i + c) % 4
                P.pe(G([MM(PS[bank], BA[:, k, i * 128:(i + 1) * 128], WBIG[:, k, c * 512:(c + 1) * 512], start=(k == 0), stop=(k == 7)) for k in range(8)]),
                     r=ba_keys(i * 128, (i + 1) * 128) + ["WBIG"], w=[psk(bank)])
                if c == 0:
                    P.act(I("copy", out=BV[:, i, 0:512], in_=PS[bank]), w=[psk(bank), ("BV", i)])
                else:
                    P.dve(I("tensor_copy", out=BV[:, i, 512:1024], in_=PS[bank]), w=[psk(bank), ("BV", i)])
        P.barrier()
        ar.release(m2)
        NU = 4
        Eall = ar.alloc([NU, 2, 512], F32)
        SPall = ar.alloc([NU, 2, 512], BF16)
        Xall = ar.alloc([2, 2, 512], F32)
        Aall = ar.alloc([3, 2, 512], BF16)
        m3 = ar.mark()
        units = []
        for g in range(NG):
            kbs = list(range(4 * g + 3, -1, -1))
            for hp_ in range(0, 8, 2):
                for ii, kb in enumerate(kbs):
                    units.append(dict(h=hp_, g=g, kb=kb, first=(ii == 0), last=(ii == len(kbs) - 1)))
        nu_ = len(units)

        def geo(u):
            jl = max(0, u["kb"] - 4 * u["g"])
            return jl * 128, 512 - jl * 128, u["kb"] >= 4 * u["g"]

        def uZ(m):
            u = units[m]
            q0, N, diag = geo(u)
            h, g, kb = u["h"], u["g"], u["kb"]
            z0 = 2 * (m % 2)
            P.pe(G([MM(PS[z0 + c][:, 0:N], BK[:, h + c, kb * 128:(kb + 1) * 128], BQ[:, h + c, g * 512 + q0:(g + 1) * 512]) for c in range(2)]),
                 r=[("BK", h), ("BK", h + 1), ("BQ", h), ("BQ", h + 1)], w=[("psZ", m % 2)])

        def uE(m):
            u = units[m]
            q0, N, diag = geo(u)
            z0 = 2 * (m % 2)
            e_, sp_ = Eall[:, m % NU, :, 0:N], SPall[:, m % NU, :, 0:N]
            ke, ksp = ("E", m % NU), ("SP", m % NU)
            P.act(I("activation", out=e_, in_=psum_t[:, z0:z0 + 2, 0:N], func=AF.Exp, scale=QK_SCALE), w=[("psZ", m % 2), ke])
            P.act(I("activation", out=sp_, in_=e_, func=AF.Ln, bias=onesF[:, 0:1], scale=1.0), r=[ke, "onesF"], w=[ksp])
            if diag:
                P.pool(I("tensor_tensor", out=SPall[:, m % NU, :, 0:128], in0=SPall[:, m % NU, :, 0:128], in1=causF.unsqueeze(1).to_broadcast([128, 2, 128]), op=ALU.mult),
                       r=["causF"], w=[ksp])

        def uRa(m):
            u = units[m]
            q0, N, diag = geo(u)
            P.pe(G([MM(PS[6 + c][:, q0:512], triB, SPall[:, m % NU, c, 0:N], start=u["first"], stop=False, skip=True) for c in range(2)]),
                 r=[("SP", m % NU), "triB"], w=["psR"])

        def uX(m):
            u = units[m]
            q0, N, diag = geo(u)
            P.act(I("activation", out=Xall[:, m % 2, :, 0:N], in_=psum_t[:, 6:8, q0:512], func=AF.Exp, scale=-1.0), w=["psR", ("X", m % 2)])

        def uRbA(m):
            u = units[m]
            q0, N, diag = geo(u)
            if not u["last"]:
                P.pe(G([MM(PS[6 + c][:, q0:512], u2B, SPall[:, m % NU, c, 0:N], start=False, stop=False, skip=True) for c in range(2)]),
                     r=[("SP", m % NU), "u2B"], w=["psR"])
            P.dve(I("tensor_tensor", out=Aall[:, m % 3, :, 0:N], in0=Eall[:, m % NU, :, 0:N], in1=Xall[:, m % 2, :, 0:N], op=ALU.mult),
                  r=[("E", m % NU), ("X", m % 2)], w=[("A", m % 3)])
            if diag:
                P.pool(I("tensor_tensor", out=Aall[:, m % 3, :, 0:128], in0=Aall[:, m % 3, :, 0:128], in1=causF.unsqueeze(1).to_broadcast([128, 2, 128]), op=ALU.mult),
                       r=["causF"], w=[("A", m % 3)])

        def uO(m):
            u = units[m]
            q0, N, diag = geo(u)
            h, g, kb = u["h"], u["g"], u["kb"]
            P.pe(G([MM(PS[4 + c][:, q0:512], BV[:, kb, (h + c) * 128:(h + c + 1) * 128], Aall[:, m % 3, c, 0:N], start=u["first"], stop=u["last"], skip=True) for c in range(2)]),
                 r=[("A", m % 3), ("BV", kb)], w=["psO"])
            if u["last"]:
                P.dve(I("tensor_copy", out=BA[:, h:h + 2, g * 512:(g + 1) * 512], in_=psum_t[:, 4:6, :]), w=["psO", "oT"])

        for n in range(nu_ + 4):
            if n < nu_:
                uZ(n)
            if 0 <= n - 1 < nu_:
                uE(n - 1)
            if 0 <= n - 3 < nu_:
                uRbA(n - 3)
            if 0 <= n - 2 < nu_:
                uRa(n - 2)
                uX(n - 2)
            if 0 <= n - 4 < nu_:
                uO(n - 4)
        P.barrier()
        ar.release(m2)
        P.dma(I("dma_start", out=WBIG, in_=b_w_out.rearrange("(k p) c -> p k c", p=128)), w=["WBIG"], q="pool")
        gb = ar.alloc([2, D], F32)
        load_gb(gb, 2)
        xs2 = [ar.alloc([D], F32) for _ in range(3)]
        pres = [ar.alloc([D], F32) for _ in range(5)]
        smalls = [ln_smalls() for _ in range(5)]
        out_proj_ln(BA, WBIG, h2, h3, tok0, 2, xs2, pres, gb, smalls)
        P.barrier()

    def moe(layer, src, dst, lnidx):
        SG = S
        NTS = SG // 128
        for sgi in range(NT // SG):
            tok0 = sgi * SG
            ar.release(base_mark)
            hTb = ar.alloc([8, SG], BF16)
            ysb = ar.alloc([NTS, D], F32)
            gates = ar.alloc([NTS, NE], F32)
            Wg = [ar.alloc([8, DFF], BF16) for _ in range(2)]
            Wu = [ar.alloc([8, DFF], BF16) for _ in range(2)]
            Wd = [ar.alloc([2, D], BF16) for _ in range(2)]
            gb = ar.alloc([2, D], F32)
            load_gb(gb, lnidx)
            m2 = ar.mark()
            hs = [ar.alloc([D], F32) for _ in range(3)]
            hT32 = [ar.alloc([8, 128], F32) for _ in range(3)]
            sgt = [ar.alloc([512], F32) for _ in range(2)]
            he = [ar.alloc([2, 512], BF16) for _ in range(3)]
            stg = [ar.alloc([8, DFF], F32), ar.alloc([8, DFF], F32), ar.alloc([2, D], F32)]
            lg_all = ar.alloc([NTS, NE], F32)
            ex = ar.alloc([NTS, NE], F32)
            pr = ar.alloc([NTS, NE], F32)
            sel = ar.alloc([NTS, NE], F32)
            gt = ar.alloc([NTS, NE], F32)
            mx = ar.alloc([NTS], F32)
            se = ar.alloc([NTS], F32)
            q4 = [ar.alloc([NTS, 4], F32) for _ in range(8)]
            bst_all = ar.alloc([NTS, 2, 6], F32)
            mv_all = ar.alloc([NTS, 2], F32)
            rstd_all = ar.alloc([NTS], F32)
            nmr_all = ar.alloc([NTS], F32)
            def r0(i):
                hb, kh = hs[i % 3], ("hs", i % 3)
                h32, k32 = hT32[i % 3], ("hT32", i % 3)
                P.dma(I("dma_start", out=hb, in_=src[tok0 + i * 128: tok0 + (i + 1) * 128, :]), w=[kh])
                ba, bb = (0, 1) if i % 2 == 0 else (2, 3)
                calls = []
                for k in range(8):
                    bank = ba if k < 4 else bb
                    calls.append(("transpose", dict(out=PS[bank][:, (k % 4) * 128:(k % 4 + 1) * 128], in_=hb[:, k * 128:(k + 1) * 128], identity=identF)))
                P.pe(G(calls), r=[kh, "identF"], w=[psk(ba), psk(bb)])
                P.act(I("copy", out=h32[:, 0:4, :], in_=PS[ba].rearrange("p (a b) -> p a b", a=4)), w=[psk(ba), (k32, 0)])
                P.dve(I("tensor_copy", out=h32[:, 4:8, :], in_=PS[bb].rearrange("p (a b) -> p a b", a=4)), w=[psk(bb), (k32, 1)])

            def r1(i):
                hb, kh = hs[i % 3], ("hs", i % 3)
                h32, k32 = hT32[i % 3], ("hT32", i % 3)
                P.pool(I("tensor_copy", out=hTb[:, :, i * 128:(i + 1) * 128], in_=h32), r=[(k32, 0), (k32, 1)], w=[("hTb", i)])
                P.pool(I("tensor_scalar", out=ysb[:, i, :], in0=hb, scalar1=ALPHA, scalar2=None, op0=ALU.mult), r=[kh], w=[("ysb", i)])
                bk = 4 + i % 2
                P.pe(G([MM(PS[bk][:, 0:NE], h32[:, k, :], rw32[:, k, :], start=(k == 0), stop=(k == 7)) for k in range(8)]), r=[(k32, 0), (k32, 1), "rw32"], w=[psk(bk)])
                P.dve(I("tensor_copy", out=lg_all[:, i, :], in_=PS[bk][:, 0:NE]), w=[psk(bk), ("lg", i)])
            for n in range(NTS + 1):
                if n < NTS:
                    r0(n)
                if n >= 1:
                    r1(n - 1)
            kg = "gat"
            lgk = [("lg", i) for i in range(NTS)]
            B3 = [128, NTS, NE]
            B4 = [128, NTS, 4, 4]

            def v4(t):
                return t.rearrange("p n (a b) -> p n a b", a=4)
            P.dve(I("tensor_reduce", out=mx, in_=lg_all, axis=AX.X, op=ALU.max), r=lgk, w=[kg])
            P.dve(I("tensor_tensor", out=ex, in0=lg_all, in1=mx.unsqueeze(2).to_broadcast(B3), op=ALU.subtract), r=lgk, w=[kg])
            P.act(I("activation", out=ex, in_=ex, func=AF.Exp), w=[kg])
            P.dve(I("tensor_reduce", out=se, in_=ex, axis=AX.X, op=ALU.add), w=[kg])
            P.dve(I("reciprocal", out=se, in_=se), w=[kg])
            P.dve(I("tensor_tensor", out=pr, in0=ex, in1=se.unsqueeze(2).to_broadcast(B3), op=ALU.mult), w=[kg])
            P.dve(I("tensor_tensor", out=sel, in0=pr, in1=rbias.unsqueeze(1).to_broadcast(B3), op=ALU.add), r=["rbias"], w=[kg])
            s4 = v4(sel)
            a_, b_, c_, d_ = s4[:, :, :, 0], s4[:, :, :, 1], s4[:, :, :, 2], s4[:, :, :, 3]
            P.dve(I("tensor_tensor", out=q4[0], in0=a_, in1=b_, op=ALU.max), w=[kg])
            P.dve(I("tensor_tensor", out=q4[1], in0=a_, in1=b_, op=ALU.min), w=[kg])
            P.dve(I("tensor_tensor", out=q4[2], in0=c_, in1=d_, op=ALU.max), w=[kg])
            P.dve(I("tensor_tensor", out=q4[3], in0=c_, in1=d_, op=ALU.min), w=[kg])
            P.dve(I("tensor_tensor", out=q4[4], in0=q4[0], in1=q4[2], op=ALU.max), w=[kg])
            P.dve(I("tensor_tensor", out=q4[5], in0=q4[0], in1=q4[2], op=ALU.min), w=[kg])
            P.dve(I("tensor_tensor", out=q4[6], in0=q4[1], in1=q4[3], op=ALU.max), w=[kg])
            P.dve(I("tensor_tensor", out=q4[7], in0=q4[5], in1=q4[6], op=ALU.max), w=[kg])
            P.dve(I("tensor_tensor", out=q4[0], in0=q4[4], in1=q4[7], op=ALU.add), w=[kg])
            P.dve(I("tensor_reduce", out=mx, in_=q4[0], axis=AX.X, op=ALU.max), w=[kg])
            P.dve(I("tensor_tensor", out=q4[1], in0=q4[0], in1=mx.unsqueeze(2).to_broadcast([128, NTS, 4]), op=ALU.is_ge), w=[kg])
            P.dve(I("tensor_tensor", out=v4(gt), in0=s4, in1=q4[7].unsqueeze(3).to_broadcast(B4), op=ALU.is_ge), w=[kg])
            P.dve(I("tensor_tensor", out=v4(gt), in0=v4(gt), in1=q4[1].unsqueeze(3).to_broadcast(B4), op=ALU.mult), w=[kg])
            P.dve(I("tensor_tensor", out=gt, in0=gt, in1=pr, op=ALU.mult), w=[kg])
            P.dve(I("tensor_reduce", out=se, in_=gt, axis=AX.X, op=ALU.add), w=[kg])
            P.dve(I("reciprocal", out=se, in_=se), w=[kg])
            P.dve(I("tensor_tensor", out=gates, in0=gt, in1=se.unsqueeze(2).to_broadcast(B3), op=ALU.mult), w=[kg, "gates"])
            steps = [(e, tg) for e in range(NE) for tg in range(SG // 512)]
            nY = [0]

            def load_w(e):
                kw = ("Wexp", e % 2)
                P.dma(I("dma_start", out=stg[0], in_=w_gate[layer, e].rearrange("(k p) f -> p k f", p=128)), w=[("stg", 0)])
                P.pool(I("tensor_copy", out=Wg[e % 2], in_=stg[0]), r=[("stg", 0)], w=[kw])
                P.dma(I("dma_start", out=stg[1], in_=w_up[layer, e].rearrange("(k p) f -> p k f", p=128)), w=[("stg", 1)])
                P.pool(I("tensor_copy", out=Wu[e % 2], in_=stg[1]), r=[("stg", 1)], w=[kw])
                P.dma(I("dma_start", out=stg[2], in_=w_down[layer, e].rearrange("(c p) d -> p c d", p=128)), w=[("stg", 2)])
                P.pool(I("tensor_copy", out=Wd[e % 2], in_=stg[2]), r=[("stg", 2)], w=[kw])

            def gu(n):
                e, tg = steps[n]
                wg, wu = Wg[e % 2], Wu[e % 2]
                kw = ("Wexp", e % 2)
                c0, c1 = tg * 512, (tg + 1) * 512
                heb = he[n % 3]
                khe = ("he", n % 3)
                hk = [("hTb", t) for t in range(tg * 4, tg * 4 + 4)]
                for fc in range(2):
                    pg, pu = 2 * fc, 2 * fc + 1
                    st_ = sgt[fc]
                    kst = ("sgt", fc)
                    P.pe(G([MM(PS[pg], wg[:, k, fc * 128:(fc + 1) * 128], hTb[:, k, c0:c1], start=(k == 0), stop=(k == 7)) for k in range(8)]), r=[kw] + hk, w=[psk(pg)])
                    P.pe(G([MM(PS[pu], wu[:, k, fc * 128:(fc + 1) * 128], hTb[:, k, c0:c1], start=(k == 0), stop=(k == 7)) for k in range(8)]), r=[kw] + hk, w=[psk(pu)])
                    P.act(I("activation", out=st_, in_=PS[pg], func=AF.Silu), w=[psk(pg), kst])
                    P.dve(I("tensor_tensor", out=heb[:, fc, :], in0=st_, in1=PS[pu], op=ALU.mult), r=[kst], w=[psk(pu), khe])

            def down(n):
                e, tg = steps[n]
                wd = Wd[e % 2]
                kw = ("Wexp", e % 2)
                heb = he[n % 3]
                khe = ("he", n % 3)
                for jt in range(4):
                    i = tg * 4 + jt
                    for c in range(2):
                        by = 4 + nY[0] % 4
                        nY[0] += 1
                        P.pe(G([MM(PS[by], heb[:, fc, jt * 128:(jt + 1) * 128], wd[:, fc, c * 512:(c + 1) * 512], start=(fc == 0), stop=(fc == 1)) for fc in range(2)]),
                             r=[khe, kw], w=[psk(by)])
                        P.dve(I("scalar_tensor_tensor", out=ysb[:, i, c * 512:(c + 1) * 512], in0=PS[by], scalar=gates[:, i, e:e + 1], in1=ysb[:, i, c * 512:(c + 1) * 512], op0=ALU.mult, op1=ALU.add),
                              r=["gates"], w=[psk(by), ("ysb", i)])
                if tg == SG // 512 - 1 and e + 2 < NE:
                    load_w(e + 2)
            load_w(0)
            load_w(1)
            gu(0)
            for n in range(len(steps)):
                if n + 1 < len(steps):
                    gu(n + 1)
                down(n)
            for i in range(NTS):
                P.dve(I("bn_stats", out=bst_all[:, i, 0, :], in_=ysb[:, i, 0:512]), r=[("ysb", i)], w=[("bst", i, 0)])
                P.dve(I("bn_stats", out=bst_all[:, i, 1, :], in_=ysb[:, i, 512:1024]), r=[("ysb", i)], w=[("bst", i, 1)])
                P.dve(I("bn_aggr", out=mv_all[:, i, :], in_=bst_all[:, i]), r=[("bst", i, 0), ("bst", i, 1)], w=[("mv", i)])
            mvk = [("mv", i) for i in range(NTS)]
            P.act(I("activation", out=rstd_all, in_=mv_all[:, :, 1], func=AF.Ln, bias=epsT[:, 0:1], scale=1.0), r=mvk + ["epsT"], w=["rstd"])
            P.act(I("activation", out=rstd_all, in_=rstd_all, func=AF.Exp, scale=-0.5), w=["rstd"])
            P.dve(I("scalar_tensor_tensor", out=nmr_all, in0=mv_all[:, :, 0], scalar=-1.0, in1=rstd_all, op0=ALU.mult, op1=ALU.mult), r=mvk + ["rstd"], w=["nmr"])
            for i in range(NTS):
                kp = ("ysb", i)
                yi = ysb[:, i, :]
                P.act(I("activation", out=yi, in_=yi, func=AF.Identity, scale=rstd_all[:, i:i + 1], bias=nmr_all[:, i:i + 1]), r=["rstd", "nmr"], w=[kp])
                P.pool(I("tensor_tensor", out=yi, in0=yi, in1=gb[:, 0, :], op=ALU.mult), r=["gb"], w=[kp])
                P.pool(I("tensor_tensor", out=yi, in0=yi, in1=gb[:, 1, :], op=ALU.add), r=["gb"], w=[kp])
                P.dma(I("dma_start", out=dst[tok0 + i * 128: tok0 + (i + 1) * 128, :], in_=yi), r=[kp], w=[("dst", i)])
            P.barrier()

    stages = [("A", lambda: [mixer_A(b) for b in range(NSEQ)]),
              ("M0", lambda: moe(0, h1, h2, 1)),
              ("B", lambda: [mixer_B(b) for b in range(NSEQ)]),
              ("M1", lambda: moe(1, h3, out, 3))]
    try:
        for name, fn in stages:
            fn()
            if stop_after == name:
                break
    except _Stop:
        pass
    P.emit(nc)
    es.close()
    nc._prog_stats = {e: len(P.eng_ops[e]) for e in ENGS}
    return nc


def rope_tables(S):
    def tab(dim, rep):
        inv = (1.0 / (10000.0 ** (np.arange(0, dim, 2, dtype=np.float32) / np.float32(dim)))).astype(np.float32)
        ang = (np.arange(S, dtype=np.float32)[:, None] * inv[None, :]).astype(np.float32)
        c = np.cos(ang).astype(np.float32).T
        s = np.sin(ang).astype(np.float32).T
        cosT = np.concatenate([c, c], 0)
        sinT = np.concatenate([-s, s], 0)
        return np.tile(cosT, (rep, 1)), np.tile(sinT, (rep, 1))
    c128, s128 = tab(128, 1)
    c64, s64 = tab(64, 2)
    return np.ascontiguousarray(np.stack([c128, s128, c64, s64], 0).astype(np.float32))


def host_consts(S):
    ii = np.arange(128)
    return {
        "c_ident": np.eye(128, dtype=np.float32),
        "c_tri": (ii[:, None] >= ii[None, :]).astype(np.float32),
        "c_caus": (ii[:, None] < ii[None, :]).astype(np.float32),
        "c_rope": rope_tables(S),
    }


def make_in_maps(inputs, S, NSEQ, ncores):
    x = np.asarray(inputs["x"], dtype=np.float32)
    cols = a_ext_cols()
    shared = {
        "w_in_ext": np.ascontiguousarray(np.asarray(inputs["a_w_in"], np.float32)[0][:, cols]),
        "a_w_out": np.ascontiguousarray(np.asarray(inputs["a_w_out"], np.float32)[0]),
        "b_w_q": np.ascontiguousarray(np.asarray(inputs["b_w_q"], np.float32)[0]),
        "b_w_kv": np.ascontiguousarray(np.asarray(inputs["b_w_kv"], np.float32)),
        "b_w_out": np.ascontiguousarray(np.asarray(inputs["b_w_out"], np.float32)[0]),
        "router_w": np.ascontiguousarray(np.asarray(inputs["router_w"], np.float32)),
        "router_bias": np.ascontiguousarray(np.asarray(inputs["router_bias"], np.float32).reshape(1, NE)),
        "exp_w_gate": np.ascontiguousarray(np.asarray(inputs["exp_w_gate"], np.float32)),
        "exp_w_up": np.ascontiguousarray(np.asarray(inputs["exp_w_up"], np.float32)),
        "exp_w_down": np.ascontiguousarray(np.asarray(inputs["exp_w_down"], np.float32)),
        "ln_g": np.ascontiguousarray(np.asarray(inputs["ln_g"], np.float32).reshape(4, D)),
        "ln_b": np.ascontiguousarray(np.asarray(inputs["ln_b"], np.float32).reshape(4, D)),
    }
    shared.update(host_consts(S))
    maps = []
    for c in range(ncores):
        m = dict(shared)
        m["x"] = np.ascontiguousarray(x[c * NSEQ:(c + 1) * NSEQ].reshape(NSEQ * S, D))
        maps.append(m)
    return maps


_NC_CACHE = {}


def kernel(**inputs):
    x = np.asarray(inputs["x"])
    B, S, _ = x.shape
    ncores = 8
    NSEQ = B // ncores
    key = (S, NSEQ)
    if key not in _NC_CACHE:
        _NC_CACHE[key] = build(S, NSEQ, min(256, S // 4))
    nc = _NC_CACHE[key]
    maps = make_in_maps(inputs, S, NSEQ, ncores)
    res = run_bass_kernel_spmd(nc, maps, core_ids=list(range(ncores)))
    outs = [np.asarray(r["out"], dtype=np.float32).reshape(NSEQ, S, D) for r in res.results]
    return np.concatenate(outs, axis=0)
```
